# Optimizing a Trainium2 kernel written in Bass

```python
import math
import jax
import jax.numpy as jnp
from jax import lax
import numpy as np

D_MODEL = 2048
BATCH = 8
SEQ = 2048
DEPTH = 2

N_META = 16
BLOCK = 128
PREFIX = BLOCK
PAD = PREFIX - N_META
HEAD_DIM = 64
N_Q_HEADS = D_MODEL // 128
N_KV_HEADS = N_Q_HEADS // 4
GQA_GROUP = N_Q_HEADS // N_KV_HEADS
ATTN_WIDTH = N_Q_HEADS * HEAD_DIM
KV_WIDTH = N_KV_HEADS * HEAD_DIM
WINDOW = 128
NUM_BUCKETS = 32
MAX_DISTANCE = 128
ML_HEADS = 4
ML_V_WIDTH = D_MODEL // 2
ML_V_DIM = ML_V_WIDTH // ML_HEADS
ML_QK_DIM = ML_V_DIM // 2
ML_QK_WIDTH = ML_HEADS * ML_QK_DIM
CONV_WIDTH = 4
CHUNK = 64
D_FF = 11 * D_MODEL // 4
N_EXPERTS = 8
TOP_K = 2
D_FF_EXPERT = D_FF
MOE_BLOCK = 512
N_DENSE = (DEPTH + 1) // 2
N_MOE = DEPTH // 2
EPS = 1e-6
IN_SIZES = (ATTN_WIDTH, KV_WIDTH, KV_WIDTH, ML_QK_WIDTH, ML_QK_WIDTH, ML_V_WIDTH, ML_HEADS, ML_HEADS, ML_V_WIDTH, D_MODEL, D_MODEL)
D_IN = sum(IN_SIZES)
IN_OFFSETS = tuple(int(v) for v in np.cumsum(IN_SIZES)[:-1])

kernel_name = 'hybrid_swa_mlstm_moe_block'


def rmsnorm(x, g):
    xf = x.astype(jnp.float32)
    y = xf * lax.rsqrt(jnp.mean(xf * xf, axis=-1, keepdims=True) + EPS)
    return (y * g.astype(jnp.float32)).astype(x.dtype)


def t5_bucket(rel):
    n = jnp.maximum(rel, 0)
    max_exact = NUM_BUCKETS // 2
    large = max_exact + (jnp.log(jnp.maximum(n, 1).astype(jnp.float32) / max_exact)
                         / math.log(MAX_DISTANCE / max_exact) * (NUM_BUCKETS - max_exact)).astype(jnp.int32)
    large = jnp.minimum(large, NUM_BUCKETS - 1)
    return jnp.where(n < max_exact, n, large)


def swa_sink_attention(q, k, v, sinks, table):
    B, T, _ = q.shape
    NB = T // BLOCK
    q = q.reshape(B, NB, BLOCK, N_KV_HEADS, GQA_GROUP, HEAD_DIM)
    k = k.reshape(B, T, N_KV_HEADS, HEAD_DIM)
    v = v.reshape(B, T, N_KV_HEADS, HEAD_DIM)
    k_meta, v_meta = k[:, PAD:PREFIX], v[:, PAD:PREFIX]
    kb = k.reshape(B, NB, BLOCK, N_KV_HEADS, HEAD_DIM)
    vb = v.reshape(B, NB, BLOCK, N_KV_HEADS, HEAD_DIM)
    shift = ((0, 0), (1, 0), (0, 0), (0, 0), (0, 0))
    k_band = jnp.concatenate([jnp.pad(kb, shift)[:, :-1], kb], axis=2)
    v_band = jnp.concatenate([jnp.pad(vb, shift)[:, :-1], vb], axis=2)
    qi = jnp.arange(BLOCK)[:, None]
    ki = jnp.arange(2 * BLOCK)[None, :]
    blk = jnp.arange(NB)[:, None, None]
    rel_band = qi + BLOCK - ki
    mask_band = (rel_band >= 0) & (rel_band < WINDOW) & ((blk - 1) * BLOCK + ki >= PAD)
    bias_band = jnp.moveaxis(table.astype(jnp.float32)[t5_bucket(rel_band)], -1, 0)
    bias_band = bias_band.reshape(N_KV_HEADS, GQA_GROUP, BLOCK, 2 * BLOCK)
    rel_meta = blk * BLOCK + qi - (PAD + jnp.arange(N_META))
    mask_meta = rel_meta >= WINDOW
    bias_meta = jnp.moveaxis(table.astype(jnp.float32)[t5_bucket(rel_meta)], -1, 0)
    bias_meta = bias_meta.reshape(N_KV_HEADS, GQA_GROUP, NB, BLOCK, N_META)
    scale = HEAD_DIM ** -0.5
    s_band = jnp.einsum('bnqhgd,bnkhd->bhgnqk', q, k_band).astype(jnp.float32) * scale + bias_band[:, :, None]
    s_band = jnp.where(mask_band, s_band, -jnp.inf)
    s_meta = jnp.einsum('bnqhgd,bmhd->bhgnqm', q, k_meta).astype(jnp.float32) * scale + bias_meta
    s_meta = jnp.where(mask_meta, s_meta, -jnp.inf)
    sink = sinks.astype(jnp.float32).reshape(N_KV_HEADS, GQA_GROUP)[None, :, :, None, None, None]
    mx = jnp.maximum(jnp.maximum(s_band.max(-1, keepdims=True), s_meta.max(-1, keepdims=True)), sink)
    p_band = jnp.exp(s_band - mx)
    p_meta = jnp.exp(s_meta - mx)
    den = p_band.sum(-1, keepdims=True) + p_meta.sum(-1, keepdims=True) + jnp.exp(sink - mx)
    o = (jnp.einsum('bhgnqk,bnkhd->bnqhgd', (p_band / den).astype(v.dtype), v_band)
         + jnp.einsum('bhgnqm,bmhd->bnqhgd', (p_meta / den).astype(v.dtype), v_meta))
    return o.reshape(B, T, ATTN_WIDTH)


def causal_conv_silu(a, w, b):
    c = a.shape[-1]
    out = lax.conv_general_dilated(a, w[:, None, :].astype(a.dtype), window_strides=(1,),
                                   padding=[(CONV_WIDTH - 1, 0)],
                                   dimension_numbers=('NWC', 'WIO', 'NWC'), feature_group_count=c)
    return jax.nn.silu(out + b.astype(a.dtype))


def mlstm_chunkwise(q, k, v, ig, lf):
    B, H, T, dk = q.shape
    dv = v.shape[-1]
    NC = T // CHUNK

    def to_chunks(a):
        return jnp.moveaxis(a.reshape(B, H, NC, CHUNK, *a.shape[3:]), 2, 0)

    causal = jnp.tril(jnp.ones((CHUNK, CHUNK), dtype=bool))

    def step(carry, inp):
        C, n, m = carry
        qc, kc, vc, igc, lfc = inp
        b = jnp.cumsum(lfc, axis=-1)
        log_d = jnp.where(causal, b[..., :, None] - b[..., None, :] + igc[..., None, :], -jnp.inf)
        m_inter = b + m[..., None]
        m_out = jnp.maximum(m_inter, log_d.max(-1))
        d = jnp.exp(log_d - m_out[..., None])
        s = jnp.einsum('bhsk,bhrk->bhsr', qc, kc) * d
        inter = jnp.exp(m_inter - m_out)
        num = s @ vc + inter[..., None] * jnp.einsum('bhvk,bhsk->bhsv', C, qc)
        den = s.sum(-1) + inter * jnp.einsum('bhk,bhsk->bhs', n, qc)
        h = num / jnp.maximum(jnp.abs(den), jnp.exp(-m_out))[..., None]
        b_last = b[..., -1]
        log_w = b_last[..., None] - b + igc
        m_new = jnp.maximum(b_last + m, log_w.max(-1))
        decay = jnp.exp(b_last + m - m_new)
        w = jnp.exp(log_w - m_new[..., None])
        C_new = decay[..., None, None] * C + jnp.einsum('bhs,bhsv,bhsk->bhvk', w, vc, kc)
        n_new = decay[..., None] * n + jnp.einsum('bhs,bhsk->bhk', w, kc)
        return (C_new, n_new, m_new), h

    init = (jnp.zeros((B, H, dv, dk), jnp.float32), jnp.zeros((B, H, dk), jnp.float32),
            jnp.zeros((B, H), jnp.float32))
    _, hs = lax.scan(step, init, (to_chunks(q), to_chunks(k), to_chunks(v), to_chunks(ig), to_chunks(lf)))
    return jnp.moveaxis(hs, 0, 2).reshape(B, H, T, dv)


def hybrid_mixer(h, w_in, sinks, conv_w, conv_b, igate_b, fgate_b, norm_g, w_attn_up, w_mlstm_up, w_out, table):
    B, T, _ = h.shape
    z = h @ w_in
    aq, ak, av, mq, mk, mv, mi, mf, mo, ga, gm = jnp.split(z, IN_OFFSETS, axis=-1)
    attn = swa_sink_attention(aq, ak, av, sinks, table)
    qk = causal_conv_silu(jnp.concatenate([mq, mk], axis=-1), conv_w, conv_b)
    mq, mk = qk[..., :ML_QK_WIDTH], qk[..., ML_QK_WIDTH:]

    def heads(a, dim):
        return a.reshape(B, T, ML_HEADS, dim).transpose(0, 2, 1, 3).astype(jnp.float32)

    valid = (jnp.arange(T) >= PAD)[:, None]
    ig = jnp.where(valid, (mi + igate_b).astype(jnp.float32), -jnp.inf)
    lf = jnp.where(valid, jax.nn.log_sigmoid((mf + fgate_b).astype(jnp.float32)), 0.0)
    hm = mlstm_chunkwise(heads(mq, ML_QK_DIM), heads(mk, ML_QK_DIM) * ML_QK_DIM ** -0.5,
                         heads(mv, ML_V_DIM), ig.transpose(0, 2, 1), lf.transpose(0, 2, 1))
    hm = hm.transpose(0, 2, 1, 3)
    hm = hm * lax.rsqrt(jnp.mean(hm * hm, axis=-1, keepdims=True) + EPS)
    hm = (hm.reshape(B, T, ML_V_WIDTH) * norm_g.astype(jnp.float32)).astype(h.dtype)
    mlstm = jax.nn.sigmoid(mo) * hm
    y = jax.nn.sigmoid(ga) * (attn @ w_attn_up) + jax.nn.sigmoid(gm) * (mlstm @ w_mlstm_up)
    return y @ w_out


def swiglu(h, w_gate, w_up, w_down):
    return (jax.nn.silu(h @ w_gate) * (h @ w_up)) @ w_down


def moe_swiglu(h, w_router, b_router, w_gate, w_up, w_down):
    n_tok, d = h.shape
    logits = h.astype(jnp.float32) @ w_router.astype(jnp.float32) + b_router.astype(jnp.float32)
    top_logit, top_e = lax.top_k(logits, TOP_K)
    top_w = jax.nn.softmax(top_logit, axis=-1)
    n_assign = n_tok * TOP_K
    e_flat = top_e.reshape(n_assign)
    tok_flat = jnp.repeat(jnp.arange(n_tok, dtype=jnp.int32), TOP_K)
    w_flat = top_w.reshape(n_assign)
    order = jnp.argsort(e_flat)
    e_sorted = e_flat[order]
    counts = jnp.bincount(e_flat, length=N_EXPERTS)
    padded = (counts + MOE_BLOCK - 1) // MOE_BLOCK * MOE_BLOCK
    starts = jnp.cumsum(counts) - counts
    pad_ends = jnp.cumsum(padded)
    pad_starts = pad_ends - padded
    dest = pad_starts[e_sorted] + jnp.arange(n_assign) - starts[e_sorted]
    n_blocks = -(-n_assign // MOE_BLOCK) + N_EXPERTS
    n_rows = n_blocks * MOE_BLOCK
    row_tok = jnp.full((n_rows,), n_tok, jnp.int32).at[dest].set(tok_flat[order])
    row_w = jnp.zeros((n_rows,), jnp.float32).at[dest].set(w_flat[order])
    blk_e = jnp.minimum(jnp.searchsorted(pad_ends, jnp.arange(n_blocks) * MOE_BLOCK, side='right'), N_EXPERTS - 1)
    h_ext = jnp.concatenate([h, jnp.zeros((1, d), h.dtype)], axis=0)
    xb = h_ext[row_tok].reshape(n_blocks, MOE_BLOCK, d)

    def expert_block(args):
        xblk, e = args
        return (jax.nn.silu(xblk @ w_gate[e]) * (xblk @ w_up[e])) @ w_down[e]

    yb = lax.map(expert_block, (xb, blk_e)).reshape(n_rows, d)
    y = jnp.zeros((n_tok + 1, d), jnp.float32).at[row_tok].add(yb.astype(jnp.float32) * row_w[:, None])
    return y[:n_tok].astype(h.dtype)


def setup_inputs(seed: int = 0) -> dict:
    key = jax.random.key(seed)
    ks = jax.random.split(key, 24)
    f32 = jnp.float32

    def nrm(k, shape, fan_in):
        return jax.random.normal(k, shape, f32) * fan_in ** -0.5

    return {
        'x': jax.random.normal(ks[0], (BATCH, SEQ, D_MODEL), f32),
        'meta_tokens': jax.random.normal(ks[1], (N_META, D_MODEL), f32),
        'rel_bias_table': 0.5 * jax.random.normal(ks[2], (NUM_BUCKETS, N_Q_HEADS), f32),
        'w_in': nrm(ks[3], (DEPTH, D_MODEL, D_IN), D_MODEL),
        'attn_sinks': jax.random.normal(ks[4], (DEPTH, N_Q_HEADS), f32),
        'conv_w': nrm(ks[5], (DEPTH, CONV_WIDTH, 2 * ML_QK_WIDTH), CONV_WIDTH),
        'conv_b': 0.02 * jax.random.normal(ks[6], (DEPTH, 2 * ML_QK_WIDTH), f32),
        'igate_b': 0.1 * jax.random.normal(ks[7], (DEPTH, ML_HEADS), f32),
        'fgate_b': jnp.linspace(3.0, 6.0, ML_HEADS, dtype=f32)[None, :] + 0.1 * jax.random.normal(ks[8], (DEPTH, ML_HEADS), f32),
        'mlstm_norm_g': 1.0 + 0.02 * jax.random.normal(ks[9], (DEPTH, ML_V_WIDTH), f32),
        'w_attn_up': nrm(ks[10], (DEPTH, ATTN_WIDTH, D_MODEL), ATTN_WIDTH),
        'w_mlstm_up': nrm(ks[11], (DEPTH, ML_V_WIDTH, D_MODEL), ML_V_WIDTH),
        'w_out': nrm(ks[12], (DEPTH, D_MODEL, D_MODEL), D_MODEL),
        'norm_mix_g': 1.0 + 0.02 * jax.random.normal(ks[13], (DEPTH, D_MODEL), f32),
        'norm_ffn_g': 1.0 + 0.02 * jax.random.normal(ks[14], (DEPTH, D_MODEL), f32),
        'w_ffn_gate': nrm(ks[15], (N_DENSE, D_MODEL, D_FF), D_MODEL),
        'w_ffn_up': nrm(ks[16], (N_DENSE, D_MODEL, D_FF), D_MODEL),
        'w_ffn_down': nrm(ks[17], (N_DENSE, D_FF, D_MODEL), D_FF),
        'w_router': nrm(ks[18], (N_MOE, D_MODEL, N_EXPERTS), D_MODEL),
        'b_router': 0.01 * jax.random.normal(ks[19], (N_MOE, N_EXPERTS), f32),
        'w_moe_gate': nrm(ks[20], (N_MOE, N_EXPERTS, D_MODEL, D_FF_EXPERT), D_MODEL),
        'w_moe_up': nrm(ks[21], (N_MOE, N_EXPERTS, D_MODEL, D_FF_EXPERT), D_MODEL),
        'w_moe_down': nrm(ks[22], (N_MOE, N_EXPERTS, D_FF_EXPERT, D_MODEL), D_FF_EXPERT),
        'final_norm_g': 1.0 + 0.02 * jax.random.normal(ks[23], (D_MODEL,), f32),
    }


def reference(x, meta_tokens, rel_bias_table, w_in, attn_sinks, conv_w, conv_b, igate_b, fgate_b,
              mlstm_norm_g, w_attn_up, w_mlstm_up, w_out, norm_mix_g, norm_ffn_g, w_ffn_gate, w_ffn_up,
              w_ffn_down, w_router, b_router, w_moe_gate, w_moe_up, w_moe_down, final_norm_g):
    B = x.shape[0]
    prefix = jnp.concatenate([jnp.zeros((B, PAD, D_MODEL), x.dtype),
                              jnp.broadcast_to(meta_tokens.astype(x.dtype), (B, N_META, D_MODEL))], axis=1)
    xs = jnp.concatenate([prefix, x], axis=1)
    for layer in range(DEPTH):
        h = rmsnorm(xs, norm_mix_g[layer])
        xs = xs + hybrid_mixer(h, w_in[layer], attn_sinks[layer], conv_w[layer], conv_b[layer],
                               igate_b[layer], fgate_b[layer], mlstm_norm_g[layer], w_attn_up[layer],
                               w_mlstm_up[layer], w_out[layer], rel_bias_table)
        h = rmsnorm(xs, norm_ffn_g[layer])
        if layer % 2 == 0:
            i = layer // 2
            ffn = swiglu(h, w_ffn_gate[i], w_ffn_up[i], w_ffn_down[i])
        else:
            i = layer // 2
            ffn = moe_swiglu(h.reshape(-1, D_MODEL), w_router[i], b_router[i], w_moe_gate[i],
                             w_moe_up[i], w_moe_down[i]).reshape(h.shape)
        xs = xs + ffn
    return rmsnorm(xs, final_norm_g)[:, PREFIX:]
```

```python
import numpy as np
import ml_dtypes
from contextlib import ExitStack
import concourse.bass as bass
import concourse.mybir as mybir
from concourse.bass_utils import run_bass_kernel_spmd

F32 = mybir.dt.float32
BF16 = mybir.dt.bfloat16
AF = mybir.ActivationFunctionType
ALU = mybir.AluOpType
AX = mybir.AxisListType

D = 2048
SEQ = 2048
T = SEQ + 128
NT = T // 128
DEPTH = 2
DFF = 5632
NFF = DFF // 128
NE = 8
CAP = 688
STILES = [(i, min(128, CAP - i)) for i in range(0, CAP, 128)]
EPS = 1e-6
NEG = -30000.0
TBLK = [(i, min(512, T - i)) for i in range(0, T, 512)]

O_AQ, O_AK, O_AV, O_MQ, O_MK, O_MV, O_MI, O_MF, O_MO, O_GA, O_GM = (
    0, 1024, 1280, 1536, 2048, 2560, 3584, 3588, 3592, 4616, 6664)


class Buf:
    __slots__ = ("w", "r")

    def __init__(self):
        self.w = None
        self.r = []


def bufs(n):
    return [Buf() for _ in range(n)]


class K:
    EPOCH = 20000
    NDSEM = 16

    def __init__(self, nc, stack):
        self.nc = nc
        self.stack = stack
        self.eng = {"pe": nc.tensor, "act": nc.scalar, "dve": nc.vector, "pool": nc.gpsimd, "sp": nc.sync}
        self.esem = {}
        self.ecnt = {}
        self.waited = {e: {} for e in self.eng}
        self.semid = 0
        self.pe_sems = set()
        self.all_esems = []
        for e in ("pe", "act", "dve", "pool"):
            self._new_esem(e)
        self.dring = {}
        for q in ("sp", "act", "pool"):
            self.dring[q] = [[self._sem("d%s%d" % (q, i)), 0] for i in range(self.NDSEM)]
        self.dpos = {q: 0 for q in self.dring}
        self.n_inst = 0
        self.uid = 0

    def _sem(self, name):
        self.semid += 1
        return self.stack.enter_context(self.nc.semaphore("%s_%d" % (name, self.semid)))

    def _new_esem(self, e):
        self.esem[e] = self._sem("e" + e)
        self.ecnt[e] = 0
        if e == "pe":
            self.pe_sems.add(id(self.esem[e]))

    def name(self, s):
        self.uid += 1
        return "%s_%d" % (s, self.uid)

    def _wait(self, e, dep):
        sem, val = dep
        w = self.waited[e]
        key = id(sem)
        if w.get(key, 0) >= val:
            return
        self.eng[e].wait_ge(sem, val)
        self.n_inst += 1
        w[key] = val

    @staticmethod
    def _deps(reads, writes):
        deps = []
        for b in reads:
            if b.w is not None:
                deps.append(b.w)
        for b in writes:
            if b.w is not None:
                deps.append(b.w)
            deps.extend(b.r)
        return deps

    @staticmethod
    def _mark(me, reads, writes):
        for b in reads:
            b.r = [x for x in b.r if x[0] is not me[0]] + [me]
        for b in writes:
            b.w = me
            b.r = []

    def op(self, e, fn, reads=(), writes=()):
        for d in self._deps(reads, writes):
            if e == "pe" and id(d[0]) in self.pe_sems:
                continue
            self._wait(e, d)
        if self.ecnt[e] >= self.EPOCH:
            self._new_esem(e)
        ins = fn(self.eng[e])
        self.ecnt[e] += 1
        ins.then_inc(self.esem[e], 1)
        self.n_inst += 1
        me = (self.esem[e], self.ecnt[e])
        self._mark(me, reads, writes)
        return me

    def dma(self, q, out, in_, reads=(), writes=()):
        ring = self.dring[q]
        pos = self.dpos[q]
        self.dpos[q] = (pos + 1) % len(ring)
        slot = ring[pos]
        if slot[1] > 0:
            self._wait(q, (slot[0], slot[1]))
        for d in self._deps(reads, writes):
            self._wait(q, d)
        ins = self.eng[q].dma_start(out=out, in_=in_)
        slot[1] += 16
        ins.then_inc(slot[0], 16)
        self.n_inst += 1
        me = (slot[0], slot[1])
        self._mark(me, reads, writes)
        return me

    def barrier(self):
        deps = []
        for q in self.dring:
            for slot in self.dring[q]:
                if slot[1] > 0:
                    deps.append((slot[0], slot[1]))
        for e in ("pe", "act", "dve", "pool"):
            if self.ecnt[e] > 0:
                deps.append((self.esem[e], self.ecnt[e]))
        for e in ("pe", "act", "dve", "pool", "sp"):
            for d in deps:
                self._wait(e, d)


class Stage:
    def __init__(self, k):
        self.k = k
        self.st = ExitStack()

    def __enter__(self):
        self.st.__enter__()
        return self

    def __exit__(self, *a):
        self.k.barrier()
        return self.st.__exit__(*a)

    def sb(self, name, shape, dt):
        return self.st.enter_context(self.k.nc.sbuf_tensor(self.k.name(name), list(shape), dt))

    def ps(self, name, shape, dt=F32):
        return self.st.enter_context(self.k.nc.psum_tensor(self.k.name(name), list(shape), dt))


def evac(k, eng, out, in_, reads, writes, func=None, scale=1.0):
    if eng == "act":
        return k.op("act", lambda e: e.activation(out=out, in_=in_, func=(func or AF.Copy), scale=scale),
                    reads=reads, writes=writes)
    assert func is None
    return k.op("dve", lambda e: e.tensor_copy(out=out, in_=in_), reads=reads, writes=writes)


def rms_tile(k, S, xt, b_xt, gbc, b_g, hb, b_hb, junk, b_junk, ss, b_ss):
    k.op("act", lambda e: e.activation(out=junk[:], in_=xt[:], func=AF.Square, accum_out=ss[:]),
         reads=[b_xt], writes=[b_junk, b_ss])
    k.op("act", lambda e: e.activation(out=ss[:], in_=ss[:], func=AF.Sqrt, scale=1.0 / D, bias=EPS),
         reads=[b_ss], writes=[b_ss])
    k.op("dve", lambda e: e.reciprocal(out=ss[:], in_=ss[:]), reads=[b_ss], writes=[b_ss])
    k.op("dve", lambda e: e.scalar_tensor_tensor(out=hb[:], in0=xt[:], scalar=ss[:], in1=gbc[:],
                                                 op0=ALU.mult, op1=ALU.mult),
         reads=[b_xt, b_ss, b_g], writes=[b_hb])


def norm_to_hT(k, S, C, xs, b_xs, g_row, hT, b_hT, tiles, tok0=0):
    gbc = S.sb("gbc", [128, D], F32); b_g = Buf()
    xt = [S.sb("xt", [128, D], F32) for _ in range(2)]; b_xt = bufs(2)
    hb = [S.sb("hb", [128, D], BF16) for _ in range(2)]; b_hb = bufs(2)
    junk = S.sb("junk", [128, D], BF16); b_junk = Buf()
    ss = [S.sb("ss", [128, 1], F32) for _ in range(2)]; b_ss = bufs(2)
    ptr = [S.ps("ptr", [128, 8, 128], BF16) for _ in range(2)]; b_ptr = bufs(2)
    k.dma("sp", gbc[:], g_row.broadcast_to([128, D]), writes=[b_g])
    tiles = list(tiles)

    def pa(i, t):
        s = i % 2
        k.dma("sp", xt[s][:], xs[t * 128:(t + 1) * 128, :], reads=[b_xs[t]], writes=[b_xt[s]])
        rms_tile(k, S, xt[s], b_xt[s], gbc, b_g, hb[s], b_hb[s], junk, b_junk, ss[s], b_ss[s])

    def pb(i, t):
        s = i % 2
        c0 = (t - tok0) * 128
        for half in range(2):
            for c in range(8):
                cc = half * 8 + c
                k.op("pe", lambda e: e.transpose(out=ptr[half][:, c, :], in_=hb[s][:, cc * 128:(cc + 1) * 128],
                                                 identity=C["identb"][:]),
                     reads=[b_hb[s], C["b"]], writes=[b_ptr[half]])
            evac(k, "act" if half == 0 else "dve", hT[:, half * 8:(half + 1) * 8, c0:c0 + 128], ptr[half][:],
                 [b_ptr[half]], [b_hT[t - tok0]])

    for i, t in enumerate(tiles):
        pa(i, t)
        if i >= 1:
            pb(i - 1, tiles[i - 1])
    pb(len(tiles) - 1, tiles[-1])


def stage_A(k, C, l, W, xs, b_xs, SC):
    w_in = W["w_in"][l]
    wv = w_in.rearrange("(kc p) n -> p kc n", p=128)
    with Stage(k) as S:
        hT = S.sb("hT", [128, 16, T], BF16); b_hT = bufs(NT)
        norm_to_hT(k, S, C, xs, b_xs, W["norm_mix_g"][l:l + 1, :], hT, b_hT, range(NT))
        ws = [S.sb("ws", [128, 16, 512], BF16) for _ in range(3)]; b_ws = bufs(3)
        pm = [S.ps("pm", [128, 512], F32) for _ in range(4)]; b_pm = bufs(4)
        sf32 = [S.sb("sf32", [128, T], F32) for _ in range(2)]; b_sf32 = bufs(2)
        sfb = [S.sb("sfb", [128, T], BF16) for _ in range(2)]; b_sfb = bufs(2)
        st32 = [S.sb("st32", [128, 512], F32) for _ in range(2)]; b_st32 = bufs(2)
        stb = [S.sb("stb", [128, 512], BF16) for _ in range(2)]; b_stb = bufs(2)
        wg = S.sb("wgate", [128, 16, 8], BF16); b_wg = Buf()
        cnt = {"w": 0, "p": 0, "f32": 0, "fb": 0, "t32": 0, "tb": 0}

        def load_w(c0, n):
            s = cnt["w"] % 3; cnt["w"] += 1
            k.dma("pool", ws[s][:, :, :n], wv[:, :, c0:c0 + n], writes=[b_ws[s]])
            return s

        def fm(s, off, M, dst, b_dst, dt, func=None):
            if dt == F32:
                i = cnt["f32"] % 2; cnt["f32"] += 1; stg, b_stg = sf32[i], b_sf32[i]
            else:
                i = cnt["fb"] % 2; cnt["fb"] += 1; stg, b_stg = sfb[i], b_sfb[i]
            for (t0, tn) in TBLK:
                p = cnt["p"] % 4; cnt["p"] += 1
                for kc in range(16):
                    lhsT = ws[s][:, kc, off:off + M] if s is not None else wg[:, kc, :]
                    k.op("pe", lambda e: e.matmul(pm[p][:M, :tn], lhsT=lhsT, rhs=hT[:, kc, t0:t0 + tn],
                                                  start=(kc == 0), stop=(kc == 15)),
                         reads=[b_ws[s] if s is not None else b_wg] + b_hT[t0 // 128:(t0 + tn) // 128], writes=[b_pm[p]])
                eng = "act" if (func is not None or p % 2 == 0) else "dve"
                evac(k, eng, stg[:M, t0:t0 + tn], pm[p][:M, :tn], [b_pm[p]], [b_stg], func=func)
            k.dma("sp", dst, stg[:M, :], reads=[b_stg], writes=[b_dst])

        def tm(s, off, n, dst, b_dst, dt, func=None):
            for t in range(NT):
                if dt == F32:
                    i = cnt["t32"] % 2; cnt["t32"] += 1; stg, b_stg = st32[i], b_st32[i]
                else:
                    i = cnt["tb"] % 2; cnt["tb"] += 1; stg, b_stg = stb[i], b_stb[i]
                p = cnt["p"] % 4; cnt["p"] += 1
                for kc in range(16):
                    k.op("pe", lambda e: e.matmul(pm[p][:, :n], lhsT=hT[:, kc, t * 128:(t + 1) * 128],
                                                  rhs=ws[s][:, kc, off:off + n], start=(kc == 0), stop=(kc == 15)),
                         reads=[b_ws[s], b_hT[t]], writes=[b_pm[p]])
                eng = "act" if (func is not None or p % 2 == 0) else "dve"
                evac(k, eng, stg[:, :n], pm[p][:, :n], [b_pm[p]], [b_stg], func=func)
                k.dma("sp", dst[t * 128:(t + 1) * 128, :], stg[:, :n], reads=[b_stg], writes=[b_dst])

        for cb in range(2):
            s = load_w(O_AQ + cb * 512, 512)
            for m in range(4):
                r0 = cb * 512 + m * 128
                fm(s, m * 128, 128, SC["qT"][r0:r0 + 128, :], SC["b_qT"], BF16)
        s = load_w(O_AK, 512)
        for m in range(2):
            fm(s, m * 128, 128, SC["kT"][m * 128:(m + 1) * 128, :], SC["b_kT"], BF16)
        tm(s, 256, 256, SC["v"], SC["b_v"], BF16)
        for cb in range(2):
            s = load_w(O_MQ + cb * 512, 512)
            for m in range(4):
                r0 = cb * 512 + m * 128
                fm(s, m * 128, 128, SC["mqk"][r0:r0 + 128, :], SC["b_mqk"], F32)
        for cb in range(2):
            s = load_w(O_MV + cb * 512, 512)
            tm(s, 0, 512, SC["mv"][:, cb * 512:(cb + 1) * 512], SC["b_mv"], BF16)
        k.dma("pool", wg[:], wv[:, :, O_MI:O_MI + 8], writes=[b_wg])
        fm(None, 0, 8, SC["gif"][:, :], SC["b_gif"], F32)
        for cb in range(2):
            s = load_w(O_MO + cb * 512, 512)
            tm(s, 0, 512, SC["smo"][:, cb * 512:(cb + 1) * 512], SC["b_smo"], F32, func=AF.Sigmoid)
        for name, off in (("sga", O_GA), ("sgm", O_GM)):
            for cb in range(4):
                s = load_w(off + cb * 512, 512)
                for m in range(4):
                    r0 = cb * 512 + m * 128
                    fm(s, m * 128, 128, SC[name][r0:r0 + 128, :], SC["b_" + name], F32, func=AF.Sigmoid)


def stage_attn(k, C, l, W, SC):
    with Stage(k) as S:
        q4 = [S.sb("q4", [64, 4, T], BF16) for _ in range(2)]; b_q4 = bufs(2)
        kj = [S.sb("kj", [64, T], BF16) for _ in range(2)]; b_kj = bufs(2)
        vj = [S.sb("vj", [128, NT, 65], BF16) for _ in range(2)]; b_vj = bufs(2)
        vm = [S.sb("vm", [16, 65], BF16) for _ in range(2)]; b_vm = bufs(2)
        oT = [S.sb("oT", [128, 2, T], BF16) for _ in range(2)]; b_oT = bufs(2)
        es = [S.sb("es", [128, 4], F32) for _ in range(2)]; b_es = bufs(2)
        ob = [S.sb("ob", [128, 4, 64], BF16) for _ in range(2)]; b_ob = bufs(2)
        for i in range(2):
            k.op("dve", lambda e: e.memset(vj[i][:, :, 64:65], 1.0), writes=[b_vj[i]])
            k.op("dve", lambda e: e.memset(vm[i][:, 64:65], 1.0), writes=[b_vm[i]])
        bpr = [[S.sb("bpr", [128, 4, 128], F32) for _ in range(2)] for _ in range(2)]
        bcu = [[S.sb("bcu", [128, 4, 128], F32) for _ in range(2)] for _ in range(2)]
        bme = [[S.sb("bme", [16, 4, 128], F32) for _ in range(2)] for _ in range(2)]
        b_bias = bufs(2)
        mpr = [S.sb("mpr", [128, 128], F32) for _ in range(2)]
        mcu = [S.sb("mcu", [128, 128], F32) for _ in range(2)]
        mme = [S.sb("mme", [16, 128], F32) for _ in range(2)]
        b_mask = Buf()
        for i in range(2):
            k.dma("sp", mpr[i][:], C["m_prev"][i], writes=[b_mask])
            k.dma("sp", mcu[i][:], C["m_cur"][i], writes=[b_mask])
            k.dma("sp", mme[i][:], C["m_meta"][i], writes=[b_mask])
        psA = [S.ps("psA", [128, 4, 128], F32) for _ in range(2)]; b_psA = bufs(2)
        psB = [S.ps("psB", [128, 4, 128], F32) for _ in range(2)]; b_psB = bufs(2)
        psC = [S.ps("psC", [16, 4, 128], F32) for _ in range(2)]; b_psC = bufs(2)
        psO = S.ps("psO", [128, 4, 128], F32); b_psO = Buf()
        ptr = S.ps("ptra", [128, 2, 128], BF16); b_ptr = Buf()
        sA = [S.sb("sA", [128, 4, 128], F32) for _ in range(2)]; b_sA = bufs(2)
        sB = [S.sb("sB", [128, 4, 128], F32) for _ in range(2)]; b_sB = bufs(2)
        sC = [S.sb("sC", [16, 4, 128], F32) for _ in range(2)]; b_sC = bufs(2)
        pA = [S.sb("pA", [128, 4, 128], BF16) for _ in range(3)]; b_pA = bufs(3)
        pB = [S.sb("pB", [128, 4, 128], BF16) for _ in range(3)]; b_pB = bufs(3)
        pC = [S.sb("pC", [16, 4, 128], BF16) for _ in range(3)]; b_pC = bufs(3)
        dn = [S.sb("dn", [128, 4], F32) for _ in range(2)]; b_dn = bufs(2)

        def load_group(j):
            s = j % 2
            k.dma("sp", q4[s][:], SC["qT"][j * 256:(j + 1) * 256, :].rearrange("(g d) t -> d g t", d=64),
                  reads=[SC["b_qT"]], writes=[b_q4[s]])
            k.dma("sp", kj[s][:], SC["kT"][j * 64:(j + 1) * 64, :], reads=[SC["b_kT"]], writes=[b_kj[s]])
            k.dma("sp", vj[s][:, :, 0:64], SC["v"][:, j * 64:(j + 1) * 64].rearrange("(n p) d -> p n d", p=128),
                  reads=[SC["b_v"]], writes=[b_vj[s]])
            k.dma("sp", vm[s][:, 0:64], SC["v"][112:128, j * 64:(j + 1) * 64], reads=[SC["b_v"]], writes=[b_vm[s]])
            k.dma("sp", es[s][:], W["attn_sinks"][l:l + 1, 4 * j:4 * j + 4].broadcast_to([128, 4]), writes=[b_es[s]])
            for i in range(2):
                for (bt, src, mk, P) in ((bpr[s][i], C["g_prev"][j], mpr[i], 128), (bcu[s][i], C["g_cur"][j], mcu[i], 128),
                                         (bme[s][i], C["g_meta"][j], mme[i], 16)):
                    k.dma("sp", bt[:], src, writes=[b_bias[s]])

        def prep_group(j):
            s = j % 2
            k.op("act", lambda e: e.activation(out=es[s][:], in_=es[s][:], func=AF.Exp), reads=[b_es[s]], writes=[b_es[s]])
            for i in range(2):
                for (bt, mk, P) in ((bpr[s][i], mpr[i], 128), (bcu[s][i], mcu[i], 128), (bme[s][i], mme[i], 16)):
                    k.op("pool", lambda e: e.tensor_tensor(out=bt[:], in0=bt[:], in1=mk[:].unsqueeze(1).broadcast_to([P, 4, 128]),
                                                           op=ALU.add), reads=[b_bias[s], b_mask], writes=[b_bias[s]])
                    k.op("act", lambda e: e.activation(out=bt[:], in_=bt[:], func=AF.Exp), reads=[b_bias[s]], writes=[b_bias[s]])

        def phase1(it, j, n):
            s = j % 2; u = it % 2; v = it % 3
            qn = q4[s][:, :, n * 128:(n + 1) * 128]
            rd = [b_q4[s], b_kj[s]]
            k.op("pe", lambda e: e.matmul(psB[u][:], lhsT=kj[s][:, n * 128:(n + 1) * 128], rhs=qn, start=True, stop=True),
                 reads=rd, writes=[b_psB[u]])
            k.op("act", lambda e: e.activation(out=sB[u][:], in_=psB[u][:], func=AF.Exp, scale=0.125), reads=[b_psB[u]], writes=[b_sB[u]])
            k.op("dve", lambda e: e.tensor_tensor(out=pB[v][:], in0=sB[u][:], in1=bcu[s][0 if n == 0 else 1][:], op=ALU.mult),
                 reads=[b_sB[u], b_bias[s]], writes=[b_pB[v]])
            if n >= 1:
                kk = 0 if n == 1 else 1
                k.op("pe", lambda e: e.matmul(psA[u][:], lhsT=kj[s][:, (n - 1) * 128:n * 128], rhs=qn, start=True, stop=True),
                     reads=rd, writes=[b_psA[u]])
                k.op("act", lambda e: e.activation(out=sA[u][:], in_=psA[u][:], func=AF.Exp, scale=0.125), reads=[b_psA[u]], writes=[b_sA[u]])
                k.op("pool", lambda e: e.tensor_tensor(out=pA[v][:], in0=sA[u][:], in1=bpr[s][kk][:], op=ALU.mult),
                     reads=[b_sA[u], b_bias[s]], writes=[b_pA[v]])
                k.op("pe", lambda e: e.matmul(psC[u][:], lhsT=kj[s][:, 112:128], rhs=qn, start=True, stop=True),
                     reads=rd, writes=[b_psC[u]])
                k.op("act", lambda e: e.activation(out=sC[u][:], in_=psC[u][:], func=AF.Exp, scale=0.125), reads=[b_psC[u]], writes=[b_sC[u]])
                k.op("pool", lambda e: e.tensor_tensor(out=pC[v][:], in0=sC[u][:], in1=bme[s][kk][:], op=ALU.mult),
                     reads=[b_sC[u], b_bias[s]], writes=[b_pC[v]])

        def phase2(it, j, n):
            s = j % 2; u = it % 2; v = it % 3
            for g in range(4):
                k.op("pe", lambda e: e.matmul(psO[:, g, 0:65], lhsT=pB[v][:, g, :], rhs=vj[s][:, n, :], start=True, stop=(n == 0)),
                     reads=[b_vj[s], b_pB[v]], writes=[b_psO])
                if n >= 1:
                    k.op("pe", lambda e: e.matmul(psO[:, g, 0:65], lhsT=pA[v][:, g, :], rhs=vj[s][:, n - 1, :], start=False, stop=False),
                         reads=[b_vj[s], b_pA[v]], writes=[b_psO])
                    k.op("pe", lambda e: e.matmul(psO[:, g, 0:65], lhsT=pC[v][:, g, :], rhs=vm[s][:], start=False, stop=True),
                         reads=[b_vm[s], b_pC[v]], writes=[b_psO])
            k.op("dve", lambda e: e.tensor_tensor(out=dn[u][:], in0=psO[:, :, 64], in1=es[s][:], op=ALU.add),
                 reads=[b_psO, b_es[s]], writes=[b_dn[u]])
            k.op("dve", lambda e: e.reciprocal(out=dn[u][:], in_=dn[u][:]), reads=[b_dn[u]], writes=[b_dn[u]])
            k.op("dve", lambda e: e.tensor_tensor(out=ob[u][:], in0=psO[:, :, 0:64],
                                                  in1=dn[u][:].unsqueeze(2).broadcast_to([128, 4, 64]), op=ALU.mult),
                 reads=[b_psO, b_dn[u]], writes=[b_ob[u]])
            obf = ob[u][:].rearrange("p g d -> p (g d)")
            for c in range(2):
                k.op("pe", lambda e: e.transpose(out=ptr[:, c, :], in_=obf[:, c * 128:(c + 1) * 128], identity=C["identb"][:]),
                     reads=[b_ob[u], C["b"]], writes=[b_ptr])
            evac(k, "act", oT[s][:, :, n * 128:(n + 1) * 128], ptr[:], [b_ptr], [b_oT[s]])
            if n == NT - 1:
                k.dma("sp", SC["attnT"][j * 256:(j + 1) * 256, :].rearrange("(c p) t -> p c t", p=128), oT[s][:],
                      reads=[b_oT[s]], writes=[SC["b_attnT"]])

        its = [(j, n) for j in range(4) for n in range(NT)]
        load_group(0)
        prep_group(0)
        for it, (j, n) in enumerate(its):
            phase1(it, j, n)
            if it >= 1:
                phase2(it - 1, *its[it - 1])
            if n == 2 and j + 1 < 4:
                load_group(j + 1)
            if n == 8 and j + 1 < 4:
                prep_group(j + 1)
        phase2(len(its) - 1, *its[-1])


def stage_mlstm(k, C, l, W, SC):
    with Stage(k) as S:
        tokq = S.sb("tokq", [128, NT, 12], F32); b_tokq = Buf()
        asc = S.sb("asc", [128, NT, 4], F32); b_asc = Buf()
        dec = S.sb("dec", [128, 4, NT], F32); b_dec = Buf()
        ngbc = S.sb("ngbc", [128, 1024], F32); b_ng = Buf()
        k.dma("sp", ngbc[:], W["mlstm_norm_g"][l:l + 1, :].broadcast_to([128, 1024]), writes=[b_ng])
        with Stage(k) as G:
            gi = G.sb("gi", [4, T], F32); gf = G.sb("gf", [4, T], F32); b_gi = Buf(); b_gf = Buf()
            ib = G.sb("ib", [4, 1], F32); fb = G.sb("fb", [4, 1], F32); b_ib = Buf(); b_fb = Buf()
            onesr = G.sb("onesr", [4, T], F32); b_or = Buf()
            Bn = G.sb("Bn", [4, T], F32); b_Bn = Buf()
            gg = G.sb("gg", [4, T], F32); b_gg = Buf()
            Mx = G.sb("Mx", [4, T], F32); b_Mx = Buf()
            Mp = G.sb("Mp", [4, NT], F32); b_Mp = Buf()
            tmp = G.sb("tmpg", [4, T], F32); b_tmp = Buf()
            qa = [G.sb("qa", [4, T], F32) for _ in range(3)]; b_qa = bufs(3)
            elast = G.sb("elast", [4, NT], F32); b_el = Buf()
            selh = G.sb("selh", [4, 4, 128], F32); b_selh = Buf()
            k.dma("sp", selh[:], C["selh"], writes=[b_selh])
            ptq = G.ps("ptq", [128, NT, 12], F32); b_ptq = Buf()
            pdec = G.ps("pdec", [128, 4, NT], F32); b_pdec = Buf()
            k.dma("sp", gi[:], SC["gif"][0:4, :], reads=[SC["b_gif"]], writes=[b_gi])
            k.dma("sp", gf[:], SC["gif"][4:8, :], reads=[SC["b_gif"]], writes=[b_gf])
            k.dma("sp", ib[:], W["igate_b"][l, :].rearrange("(h o) -> h o", o=1), writes=[b_ib])
            k.dma("sp", fb[:], W["fgate_b"][l, :].rearrange("(h o) -> h o", o=1), writes=[b_fb])
            k.op("dve", lambda e: e.memset(onesr[:], 1.0), writes=[b_or])
            k.op("dve", lambda e: e.tensor_scalar(out=fb[:], in0=fb[:], scalar1=-1.0, scalar2=None, op0=ALU.mult),
                 reads=[b_fb], writes=[b_fb])
            k.op("act", lambda e: e.activation(out=gf[:], in_=gf[:], func=AF.Exp, scale=-1.0, bias=fb[:]),
                 reads=[b_gf, b_fb], writes=[b_gf])
            k.op("act", lambda e: e.activation(out=gf[:], in_=gf[:], func=AF.Ln, scale=1.0, bias=1.0),
                 reads=[b_gf], writes=[b_gf])
            k.op("dve", lambda e: e.memset(gf[:, 0:112], 0.0), reads=[b_gf], writes=[b_gf])
            k.op("dve", lambda e: e.tensor_tensor_scan(out=Bn[:], data0=onesr[:], data1=gf[:], initial=0.0,
                                                       op0=ALU.mult, op1=ALU.add),
                 reads=[b_or, b_gf], writes=[b_Bn])
            k.op("dve", lambda e: e.scalar_tensor_tensor(out=gg[:], in0=gi[:], scalar=ib[:], in1=Bn[:],
                                                         op0=ALU.add, op1=ALU.add),
                 reads=[b_gi, b_ib, b_Bn], writes=[b_gg])
            k.op("dve", lambda e: e.memset(gg[:, 0:112], -1.0e30), reads=[b_gg], writes=[b_gg])
            k.op("dve", lambda e: e.tensor_tensor_scan(out=Mx[:], data0=onesr[:], data1=gg[:], initial=0.0,
                                                       op0=ALU.mult, op1=ALU.max),
                 reads=[b_or, b_gg], writes=[b_Mx])
            M3 = Mx[:].rearrange("p (n t) -> p n t", t=128)
            k.op("dve", lambda e: e.memset(Mp[:, 0:1], 0.0), writes=[b_Mp])
            k.op("dve", lambda e: e.tensor_copy(out=Mp[:, 1:NT], in_=M3[:, 0:NT - 1, 127]), reads=[b_Mx], writes=[b_Mp])
            Mpb = Mp[:].unsqueeze(2).broadcast_to([4, NT, 128])
            v3 = lambda t_: t_[:].rearrange("p (n t) -> p n t", t=128)
            k.op("dve", lambda e: e.tensor_tensor(out=v3(tmp), in0=v3(gg), in1=Mpb, op=ALU.subtract),
                 reads=[b_gg, b_Mp], writes=[b_tmp])
            k.op("act", lambda e: e.activation(out=qa[0][:], in_=tmp[:], func=AF.Exp), reads=[b_tmp], writes=[b_qa[0]])
            k.op("dve", lambda e: e.tensor_tensor(out=v3(tmp), in0=Mpb, in1=M3, op=ALU.subtract),
                 reads=[b_Mx, b_Mp, b_qa[0]], writes=[b_tmp])
            k.op("act", lambda e: e.activation(out=qa[1][:], in_=tmp[:], func=AF.Exp), reads=[b_tmp], writes=[b_qa[1]])
            k.op("dve", lambda e: e.tensor_tensor(out=tmp[:], in0=Bn[:], in1=Mx[:], op=ALU.subtract),
                 reads=[b_Bn, b_Mx, b_qa[1]], writes=[b_tmp])
            k.op("act", lambda e: e.activation(out=qa[2][:], in_=tmp[:], func=AF.Exp), reads=[b_tmp], writes=[b_qa[2]])
            for qi in range(3):
                for n in range(NT):
                    k.op("pe", lambda e: e.transpose(out=ptq[:, n, qi * 4:(qi + 1) * 4], in_=qa[qi][:, n * 128:(n + 1) * 128],
                                                     identity=C["identf"][0:4, 0:4]),
                         reads=[b_qa[qi], C["b"]], writes=[b_ptq])
            evac(k, "dve", tokq[:], ptq[:], [b_ptq], [b_tokq])
            k.op("dve", lambda e: e.tensor_scalar(out=asc[:], in0=tokq[:, :, 0:4], scalar1=128.0 ** -0.5, scalar2=None,
                                                  op0=ALU.mult), reads=[b_tokq], writes=[b_asc])
            k.op("dve", lambda e: e.tensor_copy(out=elast[:], in_=v3(qa[1])[:, :, 127]), reads=[b_qa[1]], writes=[b_el])
            for h in range(4):
                k.op("pe", lambda e: e.matmul(pdec[:, h, :], lhsT=selh[:, h, :], rhs=elast[:], start=True, stop=True),
                     reads=[b_el, b_selh], writes=[b_pdec])
            evac(k, "dve", dec[:], pdec[:], [b_pdec], [b_dec])
        qT = S.sb("mqT", [128, 4, T], BF16); b_qT = Buf()
        kT = S.sb("mkT", [128, 4, T], BF16); b_kT = Buf()
        ktok = S.sb("ktok", [128, NT, 512], BF16); b_ktok = Buf()
        with Stage(k) as V:
            cwb = V.sb("cwb", [128, 8, 5], F32); b_cw = Buf()
            k.dma("sp", cwb[:], W["conv_pk"][l], writes=[b_cw])
            xin = [V.sb("xin", [128, 3 + T], F32) for _ in range(2)]; b_xin = bufs(2)
            acc = [V.sb("acc", [128, T], F32) for _ in range(2)]; b_acc = bufs(2)
            pk = [V.ps("pk", [128, 4, 128], BF16) for _ in range(2)]; b_pk = bufs(2)
            for s in range(2):
                k.op("dve", lambda e: e.memset(xin[s][:, 0:3], 0.0), writes=[b_xin[s]])
            for c in range(8):
                s = c % 2
                k.dma("sp", xin[s][:, 3:3 + T], SC["mqk"][c * 128:(c + 1) * 128, :], reads=[SC["b_mqk"]], writes=[b_xin[s]])
                k.op("dve", lambda e: e.tensor_scalar(out=acc[s][:], in0=xin[s][:, 3:3 + T], scalar1=cwb[:, c, 3:4],
                                                      scalar2=cwb[:, c, 4:5], op0=ALU.mult, op1=ALU.add),
                     reads=[b_xin[s], b_cw], writes=[b_acc[s]])
                for j in range(3):
                    k.op("dve", lambda e: e.scalar_tensor_tensor(out=acc[s][:], in0=xin[s][:, j:j + T], scalar=cwb[:, c, j:j + 1],
                                                                 in1=acc[s][:], op0=ALU.mult, op1=ALU.add),
                         reads=[b_xin[s], b_cw, b_acc[s]], writes=[b_acc[s]])
                dst, b_dst = (qT, b_qT) if c < 4 else (kT, b_kT)
                k.op("act", lambda e: e.activation(out=dst[:, c % 4, :], in_=acc[s][:], func=AF.Silu),
                     reads=[b_acc[s]], writes=[b_dst])
            for n in range(NT):
                u = n % 2
                for h in range(4):
                    k.op("pe", lambda e: e.transpose(out=pk[u][:, h, :], in_=kT[:, h, n * 128:(n + 1) * 128], identity=C["identb"][:]),
                         reads=[b_kT, C["b"]], writes=[b_pk[u]])
                evac(k, "act" if u == 0 else "dve", ktok[:, n, :], pk[u][:].rearrange("p h t -> p (h t)"), [b_pk[u]], [b_ktok])
        vp = S.sb("vp", [128, NT, 4, 258], BF16); b_vp = bufs(NT)
        with Stage(k) as V2:
            mvt = V2.sb("mvt", [128, NT, 1024], BF16); b_mvt = Buf()
            k.dma("sp", mvt[:], SC["mv"].rearrange("(n p) c -> p n c", p=128), reads=[SC["b_mv"]], writes=[b_mvt])
            for n in range(NT):
                for h in range(4):
                    k.op("dve" if h % 2 == 0 else "pool",
                         lambda e: e.tensor_scalar(out=vp[:, n, h, 0:256], in0=mvt[:, n, h * 256:(h + 1) * 256],
                                                   scalar1=asc[:, n, h:h + 1], scalar2=None, op0=ALU.mult),
                         reads=[b_mvt, b_asc], writes=[b_vp[n]])
                k.op("dve", lambda e: e.tensor_copy(out=vp[:, n, :, 256:257], in_=asc[:, n, :].unsqueeze(2)),
                     reads=[b_asc], writes=[b_vp[n]])
        mT = S.sb("mT", [128, 8, T], BF16); b_mT = Buf()
        CT32 = S.sb("CT32", [128, 4, 256], F32); b_C32 = Buf()
        CTb = S.sb("CTb", [128, 4, 256], BF16); b_Cb = Buf()
        n32 = S.sb("n32", [128, 4], F32); b_n32 = Buf()
        nb = S.sb("nb", [128, 4], BF16); b_nb = Buf()
        tmpC = S.sb("tmpC", [128, 4, 256], F32); b_tmpC = Buf()
        tmpn = S.sb("tmpn", [128, 4], F32); b_tmpn = Buf()
        AT = [S.sb("AT", [128, 4, 128], BF16) for _ in range(2)]; b_AT = bufs(2)
        Gt = [S.sb("Gt", [128, 1024], F32) for _ in range(2)]; b_Gt = bufs(2)
        mt = [S.sb("mt", [128, 1024], BF16) for _ in range(2)]; b_mt = bufs(2)
        junk = S.sb("junkm", [128, 256], BF16); b_junk = Buf()
        t1 = [S.sb("t1", [128, 4], F32) for _ in range(2)]; b_t1 = bufs(2)
        r4 = [S.sb("r4", [128, 4], F32) for _ in range(2)]; b_r4 = bufs(2)
        ss4 = [S.sb("ss4", [128, 4], F32) for _ in range(2)]; b_ss4 = bufs(2)
        psA = S.ps("mpsA", [128, 4, 128], F32); b_psA = Buf()
        psO = S.ps("mpsO", [128, 4, 256], F32); b_psO = Buf()
        psC = S.ps("mpsC", [128, 4, 256], F32); b_psC = Buf()
        psD = S.ps("mpsD", [128, 8], F32); b_psD = Buf(); b_psN = Buf()
        ptm = S.ps("mptm", [128, 8, 128], BF16); b_ptm = Buf()
        k.op("dve", lambda e: e.memset(CT32[:], 0.0), writes=[b_C32])
        k.op("dve", lambda e: e.memset(CTb[:], 0.0), writes=[b_Cb])
        k.op("dve", lambda e: e.memset(n32[:], 0.0), writes=[b_n32])
        k.op("dve", lambda e: e.memset(nb[:], 0.0), writes=[b_nb])
        tri = C["tri"]
        for n in range(NT):
            u = n % 2
            tok = slice(n * 128, (n + 1) * 128)
            k.dma("sp", Gt[u][:], SC["smo"][tok, :], reads=[SC["b_smo"]], writes=[b_Gt[u]])
            k.op("pool", lambda e: e.tensor_tensor(out=Gt[u][:], in0=Gt[u][:], in1=ngbc[:], op=ALU.mult),
                 reads=[b_Gt[u], b_ng], writes=[b_Gt[u]])
            for h in range(4):
                k.op("pe", lambda e: e.matmul(psA[:, h, :], lhsT=kT[:, h, tok], rhs=qT[:, h, tok], start=True, stop=True),
                     reads=[b_kT, b_qT], writes=[b_psA])
            k.op("dve", lambda e: e.tensor_tensor(out=AT[u][:], in0=psA[:], in1=tri[:].unsqueeze(1).broadcast_to([128, 4, 128]),
                                                  op=ALU.mult),
                 reads=[b_psA, C["b"]], writes=[b_AT[u]])
            for h in range(4):
                k.op("pe", lambda e: e.matmul(psO[:, h, :], lhsT=AT[u][:, h, :], rhs=vp[:, n, h, 0:256], start=True, stop=False),
                     reads=[b_AT[u], b_vp[n]], writes=[b_psO])
                k.op("pe", lambda e: e.matmul(psO[:, h, :], lhsT=qT[:, h, tok], rhs=CTb[:, h, :], start=False, stop=True),
                     reads=[b_qT, b_Cb], writes=[b_psO])
                k.op("pe", lambda e: e.matmul(psD[:, h:h + 1], lhsT=AT[u][:, h, :], rhs=vp[:, n, h, 256:257], start=True, stop=False),
                     reads=[b_AT[u], b_vp[n]], writes=[b_psD])
                k.op("pe", lambda e: e.matmul(psD[:, h:h + 1], lhsT=qT[:, h, tok], rhs=nb[:, h:h + 1], start=False, stop=True),
                     reads=[b_qT, b_nb], writes=[b_psD])
            if n < NT - 1:
                for h in range(4):
                    k.op("pe", lambda e: e.matmul(psC[:, h, :], lhsT=ktok[:, n, h * 128:(h + 1) * 128], rhs=vp[:, n, h, 0:256],
                                                  start=True, stop=True),
                         reads=[b_ktok, b_vp[n]], writes=[b_psC])
                    k.op("pe", lambda e: e.matmul(psD[:, 4 + h:5 + h], lhsT=ktok[:, n, h * 128:(h + 1) * 128],
                                                  rhs=vp[:, n, h, 256:257], start=True, stop=True),
                         reads=[b_ktok, b_vp[n]], writes=[b_psN])
                k.op("dve", lambda e: e.tensor_tensor(out=tmpC[:], in0=psC[:], in1=CT32[:], op=ALU.add),
                     reads=[b_psC, b_C32], writes=[b_tmpC])
                k.op("dve", lambda e: e.tensor_tensor(out=CT32[:], in0=tmpC[:],
                                                      in1=dec[:, :, n].unsqueeze(2).broadcast_to([128, 4, 256]), op=ALU.mult),
                     reads=[b_tmpC, b_dec], writes=[b_C32])
                k.op("act", lambda e: e.activation(out=CTb[:], in_=CT32[:], func=AF.Copy), reads=[b_C32], writes=[b_Cb])
                k.op("dve", lambda e: e.tensor_tensor(out=tmpn[:], in0=psD[:, 4:8], in1=n32[:], op=ALU.add),
                     reads=[b_psN, b_n32], writes=[b_tmpn])
                k.op("dve", lambda e: e.tensor_tensor(out=n32[:], in0=tmpn[:], in1=dec[:, :, n], op=ALU.mult),
                     reads=[b_tmpn, b_dec], writes=[b_n32])
                k.op("act", lambda e: e.activation(out=nb[:], in_=n32[:], func=AF.Copy), reads=[b_n32], writes=[b_nb])
            k.op("dve", lambda e: e.tensor_tensor(out=t1[u][:], in0=psD[:, 0:4], in1=tokq[:, n, 4:8], op=ALU.mult),
                 reads=[b_psD, b_tokq], writes=[b_t1[u]])
            k.op("act", lambda e: e.activation(out=t1[u][:], in_=t1[u][:], func=AF.Abs),
                 reads=[b_t1[u]], writes=[b_t1[u]])
            k.op("dve", lambda e: e.tensor_tensor(out=t1[u][:], in0=t1[u][:], in1=tokq[:, n, 8:12], op=ALU.max),
                 reads=[b_t1[u], b_tokq], writes=[b_t1[u]])
            k.op("dve", lambda e: e.reciprocal(out=t1[u][:], in_=t1[u][:]), reads=[b_t1[u]], writes=[b_t1[u]])
            k.op("dve", lambda e: e.tensor_tensor(out=r4[u][:], in0=t1[u][:], in1=tokq[:, n, 4:8], op=ALU.mult),
                 reads=[b_t1[u], b_tokq], writes=[b_r4[u]])
            for h in range(4):
                k.op("act", lambda e: e.activation(out=junk[:], in_=psO[:, h, :], func=AF.Square, scale=r4[u][:, h:h + 1],
                                                   accum_out=ss4[u][:, h:h + 1]),
                     reads=[b_psO, b_r4[u]], writes=[b_junk, b_ss4[u]])
            k.op("act", lambda e: e.activation(out=ss4[u][:], in_=ss4[u][:], func=AF.Sqrt, scale=1.0 / 256, bias=EPS),
                 reads=[b_ss4[u]], writes=[b_ss4[u]])
            k.op("dve", lambda e: e.reciprocal(out=ss4[u][:], in_=ss4[u][:]), reads=[b_ss4[u]], writes=[b_ss4[u]])
            k.op("dve", lambda e: e.tensor_tensor(out=r4[u][:], in0=r4[u][:], in1=ss4[u][:], op=ALU.mult),
                 reads=[b_r4[u], b_ss4[u]], writes=[b_r4[u]])
            for h in range(4):
                k.op("dve", lambda e: e.scalar_tensor_tensor(out=mt[u][:, h * 256:(h + 1) * 256], in0=psO[:, h, :],
                                                             scalar=r4[u][:, h:h + 1], in1=Gt[u][:, h * 256:(h + 1) * 256],
                                                             op0=ALU.mult, op1=ALU.mult),
                     reads=[b_psO, b_r4[u], b_Gt[u]], writes=[b_mt[u]])
            for c in range(8):
                k.op("pe", lambda e: e.transpose(out=ptm[:, c, :], in_=mt[u][:, c * 128:(c + 1) * 128], identity=C["identb"][:]),
                     reads=[b_mt[u], C["b"]], writes=[b_ptm])
            evac(k, "act", mT[:, :, tok], ptm[:], [b_ptm], [b_mT])
        k.dma("sp", SC["mlT"].rearrange("(c p) t -> p c t", p=128), mT[:], reads=[b_mT], writes=[SC["b_mlT"]])


def t5_bucket_np(rel):
    n = np.maximum(rel, 0)
    large = 16 + (np.log(np.maximum(n, 1).astype(np.float32) / np.float32(16)) / np.float32(np.log(8.0))
                  * np.float32(16)).astype(np.int32)
    large = np.minimum(large, 31)
    return np.where(n < 16, n, large)


def pack_conv(conv_w, conv_b):
    out = np.zeros((DEPTH, 128, 8, 5), np.float32)
    for l in range(DEPTH):
        out[l, :, :, 0:4] = conv_w[l].T.reshape(8, 128, 4).transpose(1, 0, 2)
        out[l, :, :, 4] = conv_b[l].reshape(8, 128).T
    return out


def make_consts():
    c = {}
    c["identb"] = np.eye(128, dtype=np.float32).astype(ml_dtypes.bfloat16)
    c["identf"] = np.eye(128, dtype=np.float32)
    kk = np.arange(128)[:, None]
    qq = np.arange(128)[None, :]
    c["tri"] = (kk <= qq).astype(np.float32)
    selh = np.zeros((4, 4, 128), np.float32)
    for h in range(4):
        selh[h, h, :] = 1.0
    c["selh"] = selh
    mp = np.zeros((2, 128, 128), np.float32)
    mp[1] = np.where(kk > qq, 0.0, NEG)
    mp[0] = np.where((kk > qq) & (kk >= 112), 0.0, NEG)
    mc = np.zeros((2, 128, 128), np.float32)
    mc[1] = np.where(kk <= qq, 0.0, NEG)
    mc[0] = np.where((kk <= qq) & (kk >= 112), 0.0, NEG)
    mm = np.zeros((2, 16, 128), np.float32)
    m16 = np.arange(16)[:, None]
    mm[0] = np.where(qq - m16 >= 112, 0.0, NEG)
    c["m_prev"], c["m_cur"], c["m_meta"] = mp, mc, mm
    c["iota"] = np.arange(CAP, dtype=np.float32).reshape(1, CAP)
    c["trib"] = c["tri"].astype(ml_dtypes.bfloat16)
    return c


def make_gathers(table):
    kk = np.arange(128)[:, None]
    qq = np.arange(128)[None, :]
    b_prev = t5_bucket_np(qq + 128 - kk)
    b_cur = t5_bucket_np(qq - kk)
    gp = np.zeros((4, 128, 4, 128), np.float32)
    gc = np.zeros((4, 128, 4, 128), np.float32)
    gm = np.zeros((4, 16, 4, 128), np.float32)
    for j in range(4):
        for g in range(4):
            h = 4 * j + g
            gp[j, :, g, :] = table[b_prev, h]
            gc[j, :, g, :] = table[b_cur, h]
            gm[j, :, g, :] = table[31, h]
    return gp, gc, gm


CONST_SHAPES = {
    "identb": ([128, 128], BF16), "identf": ([128, 128], F32), "tri": ([128, 128], F32), "selh": ([4, 4, 128], F32),
    "m_prev": ([2, 128, 128], F32), "m_cur": ([2, 128, 128], F32), "m_meta": ([2, 16, 128], F32),
    "iota": ([1, CAP], F32), "trib": ([128, 128], BF16),
    "g_prev": ([4, 128, 4, 128], F32), "g_cur": ([4, 128, 4, 128], F32), "g_meta": ([4, 16, 4, 128], F32),
}
W_SHAPES = {
    "w_in": [DEPTH, D, 8712], "attn_sinks": [DEPTH, 16], "conv_pk": [DEPTH, 128, 8, 5], "igate_b": [DEPTH, 4],
    "fgate_b": [DEPTH, 4], "mlstm_norm_g": [DEPTH, 1024], "w_attn_up": [DEPTH, 1024, D], "w_mlstm_up": [DEPTH, 1024, D],
    "w_out": [DEPTH, D, D], "norm_mix_g": [DEPTH, D], "norm_ffn_g": [DEPTH, D], "w_ffn_gate": [1, D, DFF],
    "w_ffn_up": [1, D, DFF], "w_ffn_down": [1, DFF, D], "w_router": [1, D, NE], "b_router": [1, NE],
    "w_moe_gate": [1, NE, D, DFF], "w_moe_up": [1, NE, D, DFF], "w_moe_down": [1, NE, DFF, D], "final_norm_g": [1, D],
}
SC_SHAPES = {
    "xs": ([T, D], F32), "qT": ([1024, T], BF16), "kT": ([256, T], BF16), "v": ([T, 256], BF16),
    "mqk": ([1024, T], F32), "mv": ([T, 1024], BF16), "gif": ([8, T], F32), "smo": ([T, 1024], F32),
    "sga": ([D, T], F32), "sgm": ([D, T], F32), "attnT": ([1024, T], BF16), "mlT": ([1024, T], BF16),
    "h2b": ([16, 128, 16, 128], BF16),
}


def build(stages, feed=(), expose=(), used_w=None):
    nc = bass.Bass("TRN2", target_bir_lowering=False)
    W = {}
    for name, shp in W_SHAPES.items():
        if used_w is not None and name not in used_w:
            continue
        W[name] = nc.dram_tensor(name, shp, F32, kind="ExternalInput").ap()
    x = nc.dram_tensor("x", [SEQ, D], F32, kind="ExternalInput").ap()
    meta = nc.dram_tensor("meta", [16, D], F32, kind="ExternalInput").ap()
    CD = {n: nc.dram_tensor("c_" + n, shp, dt, kind="ExternalInput").ap() for n, (shp, dt) in CONST_SHAPES.items()}
    out = nc.dram_tensor("out", [SEQ, D], F32, kind="ExternalOutput").ap()
    dbg = nc.dram_tensor("dbg_mask", [128, 16, 8], F32, kind="ExternalOutput").ap()
    SC = {}
    for n, (shp, dt) in SC_SHAPES.items():
        kind = "ExternalInput" if n in feed else ("ExternalOutput" if n in expose else "Internal")
        SC[n] = nc.dram_tensor("s_" + n, shp, dt, kind=kind).ap()
        SC["b_" + n] = Buf()
    b_xs = bufs(NT)
    with ExitStack() as st:
        k = K(nc, st)
        C = {"b": Buf()}
        for n in ("identb", "identf", "tri"):
            shp, dt = CONST_SHAPES[n]
            C[n] = st.enter_context(nc.sbuf_tensor("C_" + n, shp, dt))
            k.dma("sp", C[n][:], CD[n], writes=[C["b"]])
        for n in ("m_prev", "m_cur", "m_meta", "g_prev", "g_cur", "g_meta", "iota", "trib", "selh"):
            C[n] = CD[n]
        for stg in stages:
            if stg == "init":
                stage_init(k, C, x, meta, SC["xs"], b_xs)
            elif stg[0] == "A":
                stage_A(k, C, int(stg[1]), W, SC["xs"], b_xs, SC)
            elif stg.startswith("attn"):
                stage_attn(k, C, int(stg[4]), W, SC)
            elif stg.startswith("ml"):
                stage_mlstm(k, C, int(stg[2]), W, SC)
            elif stg[0] == "C":
                stage_C(k, C, int(stg[1]), W, SC["xs"], b_xs, SC)
            elif stg.startswith("ffn"):
                stage_ffn_dense(k, C, int(stg[3]), W, SC["xs"], b_xs)
            elif stg == "moe":
                stage_moe(k, C, 1, W, SC["xs"], b_xs, SC, dbg, out if "final" not in stages else None)
            elif stg == "final":
                stage_final(k, C, W, SC["xs"], b_xs, out)
            else:
                raise ValueError(stg)
        k.barrier()
        print("n_inst", k.n_inst, {e: k.ecnt[e] for e in k.ecnt})
    return nc


def stage_init(k, C, x, meta, xs, b_xs):
    with Stage(k) as S:
        z = S.sb("z0", [112, D], F32); bz = Buf()
        k.op("dve", lambda e: e.memset(z[:], 0.0), writes=[bz])
        k.dma("sp", xs[0:112, :], z[:], reads=[bz], writes=[b_xs[0]])
        k.dma("sp", xs[112:128, :], meta, writes=[b_xs[0]])
        for t in range(1, NT):
            k.dma("sp", xs[t * 128:(t + 1) * 128, :], x[(t - 1) * 128:t * 128, :], writes=[b_xs[t]])


def stage_C(k, C, l, W, xs, b_xs, SC):
    with Stage(k) as S:
        yT = S.sb("yT", [128, 16, T], BF16); b_yT = Buf()
        with Stage(k) as S1:
            aT = S1.sb("aT", [128, 8, T], BF16); b_aT = Buf()
            mT = S1.sb("mT2", [128, 8, T], BF16); b_mT = Buf()
            k.dma("sp", aT[:], SC["attnT"].rearrange("(c p) t -> p c t", p=128), reads=[SC["b_attnT"]], writes=[b_aT])
            k.dma("sp", mT[:], SC["mlT"].rearrange("(c p) t -> p c t", p=128), reads=[SC["b_mlT"]], writes=[b_mT])
            wa = [S1.sb("wa", [128, 8, 512], BF16) for _ in range(2)]; b_wa = bufs(2)
            wm = [S1.sb("wm", [128, 8, 512], BF16) for _ in range(2)]; b_wm = bufs(2)
            ga = [S1.sb("ga", [128, 512], F32) for _ in range(4)]; b_ga = bufs(4)
            gm = [S1.sb("gm", [128, 512], F32) for _ in range(4)]; b_gm = bufs(4)
            ta = [S1.sb("ta", [128, 512], F32) for _ in range(2)]; b_ta = bufs(2)
            tb = [S1.sb("tb", [128, 512], F32) for _ in range(2)]; b_tb = bufs(2)
            pa = [S1.ps("pa", [128, 512], F32) for _ in range(2)]; b_pa = bufs(2)
            pb = [S1.ps("pb", [128, 512], F32) for _ in range(2)]; b_pb = bufs(2)
            wav = W["w_attn_up"][l].rearrange("(kc p) n -> p kc n", p=128)
            wmv = W["w_mlstm_up"][l].rearrange("(kc p) n -> p kc n", p=128)
            it = 0
            for fb in range(4):
                s = fb % 2
                k.dma("pool", wa[s][:], wav[:, :, fb * 512:(fb + 1) * 512], writes=[b_wa[s]])
                k.dma("pool", wm[s][:], wmv[:, :, fb * 512:(fb + 1) * 512], writes=[b_wm[s]])
                for m in range(4):
                    f = fb * 4 + m
                    for (t0, tn) in TBLK:
                        u = it % 2; g4 = it % 4; it += 1
                        k.dma("sp", ga[g4][:, :tn], SC["sga"][f * 128:(f + 1) * 128, t0:t0 + tn], reads=[SC["b_sga"]], writes=[b_ga[g4]])
                        k.dma("sp", gm[g4][:, :tn], SC["sgm"][f * 128:(f + 1) * 128, t0:t0 + tn], reads=[SC["b_sgm"]], writes=[b_gm[g4]])
                        for kc in range(8):
                            k.op("pe", lambda e: e.matmul(pa[u][:, :tn], lhsT=wa[s][:, kc, m * 128:(m + 1) * 128],
                                                          rhs=aT[:, kc, t0:t0 + tn], start=(kc == 0), stop=(kc == 7)),
                                 reads=[b_wa[s], b_aT], writes=[b_pa[u]])
                        for kc in range(8):
                            k.op("pe", lambda e: e.matmul(pb[u][:, :tn], lhsT=wm[s][:, kc, m * 128:(m + 1) * 128],
                                                          rhs=mT[:, kc, t0:t0 + tn], start=(kc == 0), stop=(kc == 7)),
                                 reads=[b_wm[s], b_mT], writes=[b_pb[u]])
                        k.op("dve", lambda e: e.tensor_tensor(out=ta[u][:, :tn], in0=pa[u][:, :tn], in1=ga[g4][:, :tn], op=ALU.mult),
                             reads=[b_pa[u], b_ga[g4]], writes=[b_ta[u]])
                        k.op("dve", lambda e: e.tensor_tensor(out=tb[u][:, :tn], in0=pb[u][:, :tn], in1=gm[g4][:, :tn], op=ALU.mult),
                             reads=[b_pb[u], b_gm[g4]], writes=[b_tb[u]])
                        k.op("pool", lambda e: e.tensor_tensor(out=yT[:, f, t0:t0 + tn], in0=ta[u][:, :tn], in1=tb[u][:, :tn], op=ALU.add),
                             reads=[b_ta[u], b_tb[u]], writes=[b_yT])
        with Stage(k) as S2:
            wo = [S2.sb("wo", [128, 16, 512], BF16) for _ in range(2)]; b_wo = bufs(2)
            xt = [S2.sb("xtc", [128, 512], F32) for _ in range(3)]; b_xt = bufs(3)
            po = [S2.ps("po", [128, 512], F32) for _ in range(3)]; b_po = bufs(3)
            wov = W["w_out"][l].rearrange("(kc p) n -> p kc n", p=128)
            it = 0
            for cb in range(4):
                s = cb % 2
                k.dma("pool", wo[s][:], wov[:, :, cb * 512:(cb + 1) * 512], writes=[b_wo[s]])
                for t in range(NT):
                    u = it % 3; it += 1
                    rows = slice(t * 128, (t + 1) * 128)
                    cols = slice(cb * 512, (cb + 1) * 512)
                    k.dma("sp", xt[u][:], xs[rows, cols], reads=[b_xs[t]], writes=[b_xt[u]])
                    for kc in range(16):
                        k.op("pe", lambda e: e.matmul(po[u][:], lhsT=yT[:, kc, rows], rhs=wo[s][:, kc, :],
                                                      start=(kc == 0), stop=(kc == 15)),
                             reads=[b_yT, b_wo[s]], writes=[b_po[u]])
                    k.op("dve", lambda e: e.tensor_tensor(out=xt[u][:], in0=po[u][:], in1=xt[u][:], op=ALU.add),
                         reads=[b_po[u], b_xt[u]], writes=[b_xt[u]])
                    k.dma("act", xs[rows, cols], xt[u][:], reads=[b_xt[u]], writes=[b_xs[t]])


def ffn_alloc(S, N):
    nsub = -(-N // 512)
    step = -(-N // nsub)
    NW = 3
    R = {"NW": NW, "step": step}
    R["wg"] = [S.sb("wg", [128, 16, 256], BF16) for _ in range(NW)]; R["b_wg"] = bufs(NW)
    R["wu"] = [S.sb("wu", [128, 16, 256], BF16) for _ in range(NW)]; R["b_wu"] = bufs(NW)
    R["wd"] = [S.sb("wd", [128, NFF, 256], BF16) for _ in range(2)]; R["b_wd"] = bufs(2)
    R["sg"] = [S.sb("sg", [128, step], F32) for _ in range(2)]; R["b_sg"] = bufs(2)
    R["psG"] = [S.ps("psG", [128, 512], F32) for _ in range(2)]; R["b_psG"] = bufs(2)
    R["psU"] = [S.ps("psU", [128, 512], F32) for _ in range(2)]; R["b_psU"] = bufs(2)
    R["psY"] = [S.ps("psY", [128, 512], F32) for _ in range(2)]; R["b_psY"] = bufs(2)
    R["cnt"] = [0, 0, 0]
    return R


def ffn_block(k, R, xT, b_xT, N, Wg, Wu, Wd, act, b_act, epilogue):
    step = R["step"]
    subs = [(i, min(step, N - i)) for i in range(0, N, step)]
    tiles = [(i, min(128, N - i)) for i in range(0, N, 128)]
    NW = R["NW"]
    wg, wu, wd, sg, psG, psU, psY = R["wg"], R["wu"], R["wd"], R["sg"], R["psG"], R["psU"], R["psY"]
    b_wg, b_wu, b_wd, b_sg, b_psG, b_psU, b_psY = R["b_wg"], R["b_wu"], R["b_wd"], R["b_sg"], R["b_psG"], R["b_psU"], R["b_psY"]
    cnt = R["cnt"]
    Wgv = Wg.rearrange("(kc p) n -> p kc n", p=128)
    Wuv = Wu.rearrange("(kc p) n -> p kc n", p=128)
    Wdv = Wd.rearrange("(fc p) n -> p fc n", p=128)
    for fg in range(NFF // 2):
        s = cnt[0] % NW; cnt[0] += 1
        k.dma("pool", wg[s][:], Wgv[:, :, fg * 256:(fg + 1) * 256], writes=[b_wg[s]])
        k.dma("pool", wu[s][:], Wuv[:, :, fg * 256:(fg + 1) * 256], writes=[b_wu[s]])
        for c in range(2):
            ffc = fg * 2 + c
            for (t0, tn) in subs:
                u = cnt[1] % 2; cnt[1] += 1
                for kc in range(16):
                    k.op("pe", lambda e: e.matmul(psG[u][:, :tn], lhsT=wg[s][:, kc, c * 128:(c + 1) * 128],
                                                  rhs=xT[:, kc, t0:t0 + tn], start=(kc == 0), stop=(kc == 15)),
                         reads=[b_wg[s], b_xT], writes=[b_psG[u]])
                for kc in range(16):
                    k.op("pe", lambda e: e.matmul(psU[u][:, :tn], lhsT=wu[s][:, kc, c * 128:(c + 1) * 128],
                                                  rhs=xT[:, kc, t0:t0 + tn], start=(kc == 0), stop=(kc == 15)),
                         reads=[b_wu[s], b_xT], writes=[b_psU[u]])
                k.op("act", lambda e: e.activation(out=sg[u][:, :tn], in_=psG[u][:, :tn], func=AF.Silu),
                     reads=[b_psG[u]], writes=[b_sg[u]])
                k.op("dve", lambda e: e.tensor_tensor(out=act[:, ffc, t0:t0 + tn], in0=sg[u][:, :tn], in1=psU[u][:, :tn], op=ALU.mult),
                     reads=[b_sg[u], b_psU[u]], writes=b_act)
    for cb in range(8):
        s = cnt[2] % 2; cnt[2] += 1
        k.dma("pool", wd[s][:], Wdv[:, :, cb * 256:(cb + 1) * 256], writes=[b_wd[s]])
        for ti, (s0, sn) in enumerate(tiles):
            u = cnt[1] % 2; cnt[1] += 1
            for ffc in range(NFF):
                k.op("pe", lambda e: e.matmul(psY[u][:sn, :256], lhsT=act[:, ffc, s0:s0 + sn], rhs=wd[s][:, ffc, :],
                                              start=(ffc == 0), stop=(ffc == NFF - 1)),
                     reads=b_act + [b_wd[s]], writes=[b_psY[u]])
            epilogue(ti, cb, psY[u][:sn, :256], b_psY[u])


def stage_ffn_dense(k, C, l, W, xs, b_xs):
    blocks = [(0, 6), (6, 6), (12, 5)]
    for (tile0, ntl) in blocks:
        N = ntl * 128
        with Stage(k) as S:
            xT = S.sb("fxT", [128, 16, N], BF16); b_xT_l = bufs(ntl)
            act = S.sb("fact", [128, NFF, N], BF16); b_act = Buf()
            with Stage(k) as S0:
                norm_to_hT(k, S0, C, xs, b_xs, W["norm_ffn_g"][l:l + 1, :], xT, b_xT_l, range(tile0, tile0 + ntl), tok0=tile0)
            b_xT = Buf()
            with Stage(k) as S1:
                xr = [S1.sb("xr", [128, 256], F32) for _ in range(3)]; b_xr = bufs(3)
                cnt = [0]

                def epi(ti, cb, ps, b_ps):
                    u = cnt[0] % 3; cnt[0] += 1
                    t = tile0 + ti
                    rows = slice(t * 128, (t + 1) * 128); cols = slice(cb * 256, (cb + 1) * 256)
                    k.dma("sp", xr[u][:], xs[rows, cols], reads=[b_xs[t]], writes=[b_xr[u]])
                    k.op("dve", lambda e: e.tensor_tensor(out=xr[u][:], in0=ps, in1=xr[u][:], op=ALU.add),
                         reads=[b_ps, b_xr[u]], writes=[b_xr[u]])
                    k.dma("act", xs[rows, cols], xr[u][:], reads=[b_xr[u]], writes=[b_xs[t]])

                R = ffn_alloc(S1, N)
                ffn_block(k, R, xT, b_xT, N, W["w_ffn_gate"][l // 2], W["w_ffn_up"][l // 2], W["w_ffn_down"][l // 2],
                          act, [b_act], epi)


def stage_moe(k, C, l, W, xs, b_xs, SC, dbg=None, out=None):
    NI = 16
    with Stage(k) as S:
        w8 = S.sb("w8", [128, NI, 8], F32); b_w8 = Buf()
        dest = S.sb("dest", [128, NI, 8], F32); b_dest = Buf()
        maskb = S.sb("maskb", [128, NI, 8], BF16); b_maskb = Buf()
        maskf = S.sb("maskf", [128, NI, 8], F32); b_maskf = Buf()
        iota = S.sb("iota", [128, CAP], F32); b_iota = Buf()
        ssF = [S.sb("ssF", [128, 1], F32) for _ in range(2)]; b_ssF = bufs(2)
        k.dma("sp", iota[:], C["iota"].broadcast_to([128, CAP]), writes=[b_iota])
        with Stage(k) as S0:
            gbc = S0.sb("gbc", [128, D], F32); b_g = Buf()
            k.dma("sp", gbc[:], W["norm_ffn_g"][l:l + 1, :].broadcast_to([128, D]), writes=[b_g])
            xt = [S0.sb("xt", [128, D], F32) for _ in range(2)]; b_xt = bufs(2)
            hf = [S0.sb("hf", [128, D], F32) for _ in range(2)]; b_hf = bufs(2)
            hb = [S0.sb("hb", [128, D], BF16) for _ in range(2)]; b_hb = bufs(2)
            junk = S0.sb("junk", [128, D], BF16); b_junk = Buf()
            ss = [S0.sb("ss", [128, 1], F32) for _ in range(2)]; b_ss = bufs(2)
            wr = S0.sb("wr", [128, 16, 8], F32); b_wr = Buf()
            br = S0.sb("br", [128, 8], F32); b_br = Buf()
            k.dma("sp", wr[:], W["w_router"][0].rearrange("(kc p) e -> p kc e", p=128), writes=[b_wr])
            k.dma("sp", br[:], W["b_router"][0:1, :].broadcast_to([128, 8]), writes=[b_br])
            hfT = [S0.sb("hfT", [128, 16, 128], F32) for _ in range(2)]; b_hfT = bufs(2)
            ptf = [S0.ps("ptf", [128, 4, 128], F32) for _ in range(2)]; b_ptf = bufs(2)
            plg = [S0.ps("plg", [128, 8], F32) for _ in range(2)]; b_plg = bufs(2)
            lg = [S0.sb("lg", [128, 8], F32) for _ in range(2)]; b_lg = bufs(2)
            mx8 = [S0.sb("mx8", [128, 8], F32) for _ in range(2)]; b_mx8 = bufs(2)
            ex = [S0.sb("ex", [128, 8], F32) for _ in range(2)]; b_ex = bufs(2)
            ntop = [S0.sb("ntop", [128, 1], F32) for _ in range(2)]; b_ntop = bufs(2)
            den = [S0.sb("den", [128, 1], F32) for _ in range(2)]; b_den = bufs(2)
            b_h2b = SC["b_h2b"]
            for i in range(NI):
                s = i % 2
                t = i + 1
                k.dma("sp", xt[s][:], xs[t * 128:(t + 1) * 128, :], reads=[b_xs[t]], writes=[b_xt[s]])
                k.op("act", lambda e: e.activation(out=junk[:], in_=xt[s][:], func=AF.Square, accum_out=ss[s][:]),
                     reads=[b_xt[s]], writes=[b_junk, b_ss[s]])
                k.op("act", lambda e: e.activation(out=ss[s][:], in_=ss[s][:], func=AF.Sqrt, scale=1.0 / D, bias=EPS),
                     reads=[b_ss[s]], writes=[b_ss[s]])
                k.op("dve", lambda e: e.reciprocal(out=ss[s][:], in_=ss[s][:]), reads=[b_ss[s]], writes=[b_ss[s]])
                k.op("dve", lambda e: e.scalar_tensor_tensor(out=hf[s][:], in0=xt[s][:], scalar=ss[s][:], in1=gbc[:],
                                                             op0=ALU.mult, op1=ALU.mult),
                     reads=[b_xt[s], b_ss[s], b_g], writes=[b_hf[s]])
                k.op("act", lambda e: e.activation(out=hb[s][:], in_=hf[s][:], func=AF.Copy), reads=[b_hf[s]], writes=[b_hb[s]])
                k.dma("sp", SC["h2b"][:, :, i, :].rearrange("c p f -> p c f"), hb[s][:].rearrange("p (c f) -> p c f", f=128),
                      reads=[b_hb[s]], writes=[b_h2b])
                for c4 in range(4):
                    u = c4 % 2
                    for c in range(4):
                        cc = c4 * 4 + c
                        k.op("pe", lambda e: e.transpose(out=ptf[u][:, c, :], in_=hf[s][:, cc * 128:(cc + 1) * 128],
                                                         identity=C["identf"][:]),
                             reads=[b_hf[s], C["b"]], writes=[b_ptf[u]])
                    evac(k, "act" if u == 0 else "dve", hfT[s][:, c4 * 4:(c4 + 1) * 4, :], ptf[u][:], [b_ptf[u]], [b_hfT[s]])
                for kc in range(16):
                    k.op("pe", lambda e: e.matmul(plg[s][:], lhsT=hfT[s][:, kc, :], rhs=wr[:, kc, :], start=(kc == 0), stop=(kc == 15)),
                         reads=[b_hfT[s], b_wr], writes=[b_plg[s]])
                k.op("dve", lambda e: e.tensor_tensor(out=lg[s][:], in0=plg[s][:], in1=br[:], op=ALU.add),
                     reads=[b_plg[s], b_br], writes=[b_lg[s]])
                k.op("dve", lambda e: e.max(out=mx8[s][:], in_=lg[s][:]), reads=[b_lg[s]], writes=[b_mx8[s]])
                k.op("dve", lambda e: e.tensor_scalar(out=maskf[:, i, :], in0=lg[s][:], scalar1=mx8[s][:, 1:2], scalar2=None,
                                                      op0=ALU.is_ge), reads=[b_lg[s], b_mx8[s]], writes=[b_maskf])
                k.op("dve", lambda e: e.tensor_scalar(out=ntop[s][:], in0=mx8[s][:, 0:1], scalar1=-1.0, scalar2=None, op0=ALU.mult),
                     reads=[b_mx8[s]], writes=[b_ntop[s]])
                k.op("act", lambda e: e.activation(out=ex[s][:], in_=lg[s][:], func=AF.Exp, bias=ntop[s][:]),
                     reads=[b_lg[s], b_ntop[s]], writes=[b_ex[s]])
                k.op("dve", lambda e: e.tensor_tensor(out=ex[s][:], in0=ex[s][:], in1=maskf[:, i, :], op=ALU.mult),
                     reads=[b_ex[s], b_maskf], writes=[b_ex[s]])
                k.op("dve", lambda e: e.reduce_sum(out=den[s][:], in_=ex[s][:], axis=AX.X), reads=[b_ex[s]], writes=[b_den[s]])
                k.op("dve", lambda e: e.reciprocal(out=den[s][:], in_=den[s][:]), reads=[b_den[s]], writes=[b_den[s]])
                k.op("dve", lambda e: e.tensor_scalar(out=w8[:, i, :], in0=ex[s][:], scalar1=den[s][:], scalar2=None, op0=ALU.mult),
                     reads=[b_ex[s], b_den[s]], writes=[b_w8])
                k.op("dve", lambda e: e.tensor_copy(out=maskb[:, i, :], in_=maskf[:, i, :]), reads=[b_maskf], writes=[b_maskb])
        if dbg is not None:
            k.dma("sp", dbg, maskf[:], reads=[b_maskf], writes=[Buf()])
        with Stage(k) as S1:
            onesb = S1.sb("onesb", [128, 128], BF16); b_ones = Buf()
            trib = S1.sb("trib", [128, 128], BF16); b_trib = Buf()
            k.op("dve", lambda e: e.memset(onesb[:], 1.0), writes=[b_ones])
            k.dma("sp", trib[:], C["trib"], writes=[b_trib])
            pcs = [S1.ps("pcs", [128, 8], F32) for _ in range(2)]; b_pcs = bufs(2)
            for i in range(NI):
                u = i % 2
                for i2 in range(i):
                    k.op("pe", lambda e: e.matmul(pcs[u][:], lhsT=onesb[:], rhs=maskb[:, i2, :], start=(i2 == 0), stop=False),
                         reads=[b_ones, b_maskb], writes=[b_pcs[u]])
                k.op("pe", lambda e: e.matmul(pcs[u][:], lhsT=trib[:], rhs=maskb[:, i, :], start=(i == 0), stop=True),
                     reads=[b_trib, b_maskb], writes=[b_pcs[u]])
                k.op("dve", lambda e: e.tensor_tensor(out=dest[:, i, :], in0=pcs[u][:], in1=maskf[:, i, :], op=ALU.mult),
                     reads=[b_pcs[u], b_maskf], writes=[b_dest])
            k.op("dve", lambda e: e.tensor_scalar(out=dest[:], in0=dest[:], scalar1=-1.0, scalar2=None, op0=ALU.add),
                 reads=[b_dest], writes=[b_dest])
        NS = len(STILES)
        SelT = S.sb("SelT", [128, NS, NI * 128], BF16); b_SelT = Buf()
        xy = S.sb("mxy", [128, max(16 * CAP, NS * D)], BF16); b_xT = Buf()
        xT = xy[:, 0:16 * CAP].rearrange("p (c n) -> p c n", n=CAP)
        ye = xy[:, 0:NS * D].rearrange("p (s d) -> p s d", d=D)
        b_ye = b_xT
        araw = S.sb("mact", [128, NFF * CAP], BF16)
        act = araw[:].rearrange("p (f n) -> p f n", n=CAP)
        o1 = NI * CAP
        Sel = araw[:, 0:o1].rearrange("p (i n) -> p i n", n=CAP)
        hfc = [araw[:, o1 + q * 2048:o1 + (q + 1) * 2048].rearrange("p (i f) -> p i f", f=128) for q in range(2)]
        o2 = o1 + 2 * 2048
        NXR = 3
        assert o2 + NXR * 2 * D <= NFF * CAP
        xr = [araw[:, o2 + q * 2 * D:o2 + (q + 1) * 2 * D].bitcast(F32) for q in range(NXR)]
        b_R1, b_R3 = Buf(), Buf()
        b_hfc = bufs(2); b_xr = bufs(NXR)
        b_act = [b_R1, b_R3] + b_hfc + b_xr
        R = ffn_alloc(S, CAP)
        ptr = [S.ps("ptrs", [128, 8, 128], BF16) for _ in range(2)]; b_ptr = bufs(2)
        psc = [R["psG"][0], R["psG"][1], R["psU"][0], R["psU"][1]]
        b_psc = [R["b_psG"][0], R["b_psG"][1], R["b_psU"][0], R["b_psU"][1]]
        jstep = -(-CAP // (-(-CAP // 512)))
        jsub = [(i, min(jstep, CAP - i)) for i in range(0, CAP, jstep)]
        itc = [0, 0, 0]
        for ex_i in range(NE):
            for i in range(NI):
                k.op("dve", lambda e: e.tensor_scalar(out=Sel[:, i, :], in0=iota[:], scalar1=dest[:, i, ex_i:ex_i + 1],
                                                      scalar2=None, op0=ALU.is_equal),
                     reads=[b_iota, b_dest], writes=[b_R1])
            for fc in range(16):
                s = fc % 2
                k.dma("sp", hfc[s], SC["h2b"][fc], reads=[SC["b_h2b"]], writes=[b_hfc[s]])
                for (j0, jn) in jsub:
                    u = itc[0] % 2; itc[0] += 1
                    pg, b_pg = R["psG"][u], R["b_psG"][u]
                    for i in range(NI):
                        k.op("pe", lambda e: e.matmul(pg[:, :jn], lhsT=hfc[s][:, i, :], rhs=Sel[:, i, j0:j0 + jn],
                                                      start=(i == 0), stop=(i == NI - 1)),
                             reads=[b_hfc[s], b_R1], writes=[b_pg])
                    evac(k, "act" if u == 0 else "dve", xT[:, fc, j0:j0 + jn], pg[:, :jn], [b_pg], [b_xT])
            for si, (s0, sn) in enumerate(STILES):
                for half in range(2):
                    u = itc[1] % 2; itc[1] += 1
                    for i8 in range(8):
                        i = half * 8 + i8
                        k.op("pe", lambda e: e.transpose(out=ptr[u][:sn, i8, :], in_=Sel[:, i, s0:s0 + sn], identity=C["identb"][:]),
                             reads=[b_R1, C["b"]], writes=[b_ptr[u]])
                    evac(k, "act" if u == 0 else "dve", SelT[:sn, si, half * 1024:(half + 1) * 1024],
                         ptr[u][:sn].rearrange("p a b -> p (a b)"), [b_ptr[u]], [b_SelT])

            def epi(ti, cb, ps, b_ps):
                sn = STILES[ti][1]
                evac(k, "act" if (ti + cb) % 2 == 0 else "dve", ye[:sn, ti, cb * 256:(cb + 1) * 256], ps, [b_ps], [b_ye])

            ffn_block(k, R, xT, b_xT, CAP, W["w_moe_gate"][0, ex_i], W["w_moe_up"][0, ex_i], W["w_moe_down"][0, ex_i],
                      act, b_act, epi)
            fuse_final = (out is not None and ex_i == NE - 1)
            if fuse_final:
                gF = R["wd"][1][:].rearrange("p a b -> p (a b)").bitcast(F32)[:, 0:D]
                jF = R["wd"][0][:].rearrange("p a b -> p (a b)")[:, 0:D]
                b_gF, b_jF = R["b_wd"][1], R["b_wd"][0]
                k.dma("sp", gF, W["final_norm_g"][0:1, :].broadcast_to([128, D]), writes=[b_gF])
                b_outF = Buf()
            for i in range(NI):
                t = i + 1
                u = itc[2] % NXR; itc[2] += 1
                rows = slice(t * 128, (t + 1) * 128)
                k.dma("sp", xr[u], xs[rows, :], reads=[b_xs[t]], writes=[b_xr[u]])
                for cb in range(4):
                    cols = slice(cb * 512, (cb + 1) * 512)
                    for si, (s0, sn) in enumerate(STILES):
                        k.op("pe", lambda e: e.matmul(psc[cb][:], lhsT=SelT[:sn, si, i * 128:(i + 1) * 128], rhs=ye[:sn, si, cols],
                                                      start=(si == 0), stop=(si == NS - 1)),
                             reads=[b_SelT, b_ye], writes=[b_psc[cb]])
                    k.op("dve", lambda e: e.scalar_tensor_tensor(out=xr[u][:, cols], in0=psc[cb][:], scalar=w8[:, i, ex_i:ex_i + 1],
                                                                 in1=xr[u][:, cols], op0=ALU.mult, op1=ALU.add),
                         reads=[b_psc[cb], b_w8, b_xr[u]], writes=[b_xr[u]])
                if not fuse_final:
                    k.dma("act", xs[rows, :], xr[u], reads=[b_xr[u]], writes=[b_xs[t]])
                else:
                    sF = ssF[i % 2]; b_sF = b_ssF[i % 2]
                    k.op("act", lambda e: e.activation(out=jF, in_=xr[u], func=AF.Square, accum_out=sF[:]),
                         reads=[b_xr[u]], writes=[b_jF, b_sF])
                    k.op("act", lambda e: e.activation(out=sF[:], in_=sF[:], func=AF.Sqrt, scale=1.0 / D, bias=EPS),
                         reads=[b_sF], writes=[b_sF])
                    k.op("dve", lambda e: e.reciprocal(out=sF[:], in_=sF[:]), reads=[b_sF], writes=[b_sF])
                    k.op("dve", lambda e: e.scalar_tensor_tensor(out=xr[u], in0=xr[u], scalar=sF[:], in1=gF, op0=ALU.mult, op1=ALU.mult),
                         reads=[b_xr[u], b_sF, b_gF], writes=[b_xr[u]])
                    k.dma("act", out[i * 128:(i + 1) * 128, :], xr[u], reads=[b_xr[u]], writes=[b_outF])


def stage_final(k, C, W, xs, b_xs, out):
    with Stage(k) as S:
        gbc = S.sb("gbc", [128, D], F32); b_g = Buf()
        k.dma("sp", gbc[:], W["final_norm_g"][0:1, :].broadcast_to([128, D]), writes=[b_g])
        xt = [S.sb("xt", [128, D], F32) for _ in range(2)]; b_xt = bufs(2)
        ot = [S.sb("ot", [128, D], F32) for _ in range(2)]; b_ot = bufs(2)
        junk = S.sb("junk", [128, D], BF16); b_junk = Buf()
        ss = [S.sb("ss", [128, 1], F32) for _ in range(2)]; b_ss = bufs(2)
        b_out = Buf()
        for i in range(16):
            s = i % 2
            t = i + 1
            k.dma("sp", xt[s][:], xs[t * 128:(t + 1) * 128, :], reads=[b_xs[t]], writes=[b_xt[s]])
            k.op("act", lambda e: e.activation(out=junk[:], in_=xt[s][:], func=AF.Square, accum_out=ss[s][:]),
                 reads=[b_xt[s]], writes=[b_junk, b_ss[s]])
            k.op("act", lambda e: e.activation(out=ss[s][:], in_=ss[s][:], func=AF.Sqrt, scale=1.0 / D, bias=EPS),
                 reads=[b_ss[s]], writes=[b_ss[s]])
            k.op("dve", lambda e: e.reciprocal(out=ss[s][:], in_=ss[s][:]), reads=[b_ss[s]], writes=[b_ss[s]])
            k.op("dve", lambda e: e.scalar_tensor_tensor(out=ot[s][:], in0=xt[s][:], scalar=ss[s][:], in1=gbc[:],
                                                         op0=ALU.mult, op1=ALU.mult),
                 reads=[b_xt[s], b_ss[s], b_g], writes=[b_ot[s]])
            k.dma("sp", out[i * 128:(i + 1) * 128, :], ot[s][:], reads=[b_ot[s]], writes=[b_out])


ALL_STAGES = ["init", "A0", "attn0", "ml0", "C0", "ffn0", "A1", "attn1", "ml1", "C1", "moe"]
_NC_CACHE = {}


def kernel(**inputs):
    f32 = lambda a: np.ascontiguousarray(np.asarray(a, dtype=np.float32))
    x = f32(inputs["x"])
    B = x.shape[0]
    shared = {}
    for name in W_SHAPES:
        if name == "conv_pk":
            shared[name] = pack_conv(f32(inputs["conv_w"]), f32(inputs["conv_b"]))
        elif name == "final_norm_g":
            shared[name] = f32(inputs["final_norm_g"]).reshape(1, D)
        else:
            shared[name] = f32(inputs[name])
    shared["meta"] = f32(inputs["meta_tokens"])
    c = make_consts()
    gp, gc, gm = make_gathers(f32(inputs["rel_bias_table"]))
    c["g_prev"], c["g_cur"], c["g_meta"] = gp, gc, gm
    for n, v in c.items():
        shared["c_" + n] = v
    if "nc" not in _NC_CACHE:
        _NC_CACHE["nc"] = build(ALL_STAGES)
    nc = _NC_CACHE["nc"]
    in_maps = []
    for b in range(B):
        m = dict(shared)
        m["x"] = x[b]
        in_maps.append(m)
    res = run_bass_kernel_spmd(nc, in_maps, core_ids=list(range(B)))
    _NC_CACHE["counts"] = [np.asarray(r["dbg_mask"]).sum(axis=(0, 1)) for r in res.results]
    return np.stack([np.asarray(r["out"], dtype=np.float32) for r in res.results], axis=0)
```

```python
import numpy as np
import ml_dtypes
from contextlib import ExitStack
import concourse.bass as bass
import concourse.mybir as mybir
from concourse.bass_utils import run_bass_kernel_spmd

F32 = mybir.dt.float32
BF16 = mybir.dt.bfloat16
AF = mybir.ActivationFunctionType
ALU = mybir.AluOpType
AX = mybir.AxisListType

D = 2048
SEQ = 2048
T = SEQ + 128
NT = T // 128
DEPTH = 2
DFF = 5632
NFF = DFF // 128
NE = 8
CAP = 688
STILES = [(i, min(128, CAP - i)) for i in range(0, CAP, 128)]
EPS = 1e-6
NEG = -30000.0
TBLK = [(i, min(512, T - i)) for i in range(0, T, 512)]

O_AQ, O_AK, O_AV, O_MQ, O_MK, O_MV, O_MI, O_MF, O_MO, O_GA, O_GM = (
    0, 1024, 1280, 1536, 2048, 2560, 3584, 3588, 3592, 4616, 6664)


class Buf:
    __slots__ = ("w", "r")

    def __init__(self):
        self.w = None
        self.r = []


def bufs(n):
    return [Buf() for _ in range(n)]


class K:
    EPOCH = 20000
    NDSEM = 16

    def __init__(self, nc, stack):
        self.nc = nc
        self.stack = stack
        self.eng = {"pe": nc.tensor, "act": nc.scalar, "dve": nc.vector, "pool": nc.gpsimd, "sp": nc.sync}
        self.esem = {}
        self.ecnt = {}
        self.waited = {e: {} for e in self.eng}
        self.semid = 0
        self.pe_sems = set()
        self.all_esems = []
        for e in ("pe", "act", "dve", "pool"):
            self._new_esem(e)
        self.dring = {}
        for q in ("sp", "act", "pool"):
            self.dring[q] = [[self._sem("d%s%d" % (q, i)), 0] for i in range(self.NDSEM)]
        self.dpos = {q: 0 for q in self.dring}
        self.n_inst = 0
        self.uid = 0

    def _sem(self, name):
        self.semid += 1
        return self.stack.enter_context(self.nc.semaphore("%s_%d" % (name, self.semid)))

    def _new_esem(self, e):
        self.esem[e] = self._sem("e" + e)
        self.ecnt[e] = 0
        if e == "pe":
            self.pe_sems.add(id(self.esem[e]))

    def name(self, s):
        self.uid += 1
        return "%s_%d" % (s, self.uid)

    def _wait(self, e, dep):
        sem, val = dep
        w = self.waited[e]
        key = id(sem)
        if w.get(key, 0) >= val:
            return
        self.eng[e].wait_ge(sem, val)
        self.n_inst += 1
        w[key] = val

    @staticmethod
    def _deps(reads, writes):
        deps = []
        for b in reads:
            if b.w is not None:
                deps.append(b.w)
        for b in writes:
            if b.w is not None:
                deps.append(b.w)
            deps.extend(b.r)
        return deps

    @staticmethod
    def _mark(me, reads, writes):
        for b in reads:
            b.r = [x for x in b.r if x[0] is not me[0]] + [me]
        for b in writes:
            b.w = me
            b.r = []

    def op(self, e, fn, reads=(), writes=()):
        for d in self._deps(reads, writes):
            if e == "pe" and id(d[0]) in self.pe_sems:
                continue
            self._wait(e, d)
        if self.ecnt[e] >= self.EPOCH:
            self._new_esem(e)
        ins = fn(self.eng[e])
        self.ecnt[e] += 1
        ins.then_inc(self.esem[e], 1)
        self.n_inst += 1
        me = (self.esem[e], self.ecnt[e])
        self._mark(me, reads, writes)
        return me

    def dma(self, q, out, in_, reads=(), writes=()):
        ring = self.dring[q]
        pos = self.dpos[q]
        self.dpos[q] = (pos + 1) % len(ring)
        slot = ring[pos]
        if slot[1] > 0:
            self._wait(q, (slot[0], slot[1]))
        for d in self._deps(reads, writes):
            self._wait(q, d)
        ins = self.eng[q].dma_start(out=out, in_=in_)
        slot[1] += 16
        ins.then_inc(slot[0], 16)
        self.n_inst += 1
        me = (slot[0], slot[1])
        self._mark(me, reads, writes)
        return me

    def barrier(self):
        deps = []
        for q in self.dring:
            for slot in self.dring[q]:
                if slot[1] > 0:
                    deps.append((slot[0], slot[1]))
        for e in ("pe", "act", "dve", "pool"):
            if self.ecnt[e] > 0:
                deps.append((self.esem[e], self.ecnt[e]))
        for e in ("pe", "act", "dve", "pool", "sp"):
            for d in deps:
                self._wait(e, d)


class Stage:
    def __init__(self, k):
        self.k = k
        self.st = ExitStack()

    def __enter__(self):
        self.st.__enter__()
        return self

    def __exit__(self, *a):
        self.k.barrier()
        return self.st.__exit__(*a)

    def sb(self, name, shape, dt):
        return self.st.enter_context(self.k.nc.sbuf_tensor(self.k.name(name), list(shape), dt))

    def ps(self, name, shape, dt=F32):
        return self.st.enter_context(self.k.nc.psum_tensor(self.k.name(name), list(shape), dt))


def evac(k, eng, out, in_, reads, writes, func=None, scale=1.0):
    if eng == "act":
        return k.op("act", lambda e: e.activation(out=out, in_=in_, func=(func or AF.Copy), scale=scale),
                    reads=reads, writes=writes)
    assert func is None
    return k.op("dve", lambda e: e.tensor_copy(out=out, in_=in_), reads=reads, writes=writes)


def rms_tile(k, S, xt, b_xt, gbc, b_g, hb, b_hb, junk, b_junk, ss, b_ss):
    k.op("act", lambda e: e.activation(out=junk[:], in_=xt[:], func=AF.Square, accum_out=ss[:]),
         reads=[b_xt], writes=[b_junk, b_ss])
    k.op("act", lambda e: e.activation(out=ss[:], in_=ss[:], func=AF.Sqrt, scale=1.0 / D, bias=EPS),
         reads=[b_ss], writes=[b_ss])
    k.op("dve", lambda e: e.reciprocal(out=ss[:], in_=ss[:]), reads=[b_ss], writes=[b_ss])
    k.op("dve", lambda e: e.scalar_tensor_tensor(out=hb[:], in0=xt[:], scalar=ss[:], in1=gbc[:],
                                                 op0=ALU.mult, op1=ALU.mult),
         reads=[b_xt, b_ss, b_g], writes=[b_hb])


def norm_to_hT(k, S, C, xs, b_xs, g_row, hT, b_hT, tiles, tok0=0):
    gbc = S.sb("gbc", [128, D], F32); b_g = Buf()
    xt = [S.sb("xt", [128, D], F32) for _ in range(2)]; b_xt = bufs(2)
    hb = [S.sb("hb", [128, D], BF16) for _ in range(2)]; b_hb = bufs(2)
    junk = S.sb("junk", [128, D], BF16); b_junk = Buf()
    ss = [S.sb("ss", [128, 1], F32) for _ in range(2)]; b_ss = bufs(2)
    ptr = [S.ps("ptr", [128, 8, 128], BF16) for _ in range(2)]; b_ptr = bufs(2)
    k.dma("sp", gbc[:], g_row.broadcast_to([128, D]), writes=[b_g])
    tiles = list(tiles)

    def pa(i, t):
        s = i % 2
        k.dma("sp", xt[s][:], xs[t * 128:(t + 1) * 128, :], reads=[b_xs[t]], writes=[b_xt[s]])
        rms_tile(k, S, xt[s], b_xt[s], gbc, b_g, hb[s], b_hb[s], junk, b_junk, ss[s], b_ss[s])

    def pb(i, t):
        s = i % 2
        c0 = (t - tok0) * 128
        for half in range(2):
            for c in range(8):
                cc = half * 8 + c
                k.op("pe", lambda e: e.transpose(out=ptr[half][:, c, :], in_=hb[s][:, cc * 128:(cc + 1) * 128],
                                                 identity=C["identb"][:]),
                     reads=[b_hb[s], C["b"]], writes=[b_ptr[half]])
            evac(k, "act" if half == 0 else "dve", hT[:, half * 8:(half + 1) * 8, c0:c0 + 128], ptr[half][:],
                 [b_ptr[half]], [b_hT[t - tok0]])

    for i, t in enumerate(tiles):
        pa(i, t)
        if i >= 1:
            pb(i - 1, tiles[i - 1])
    pb(len(tiles) - 1, tiles[-1])


def stage_A(k, C, l, W, xs, b_xs, SC):
    w_in = W["w_in"][l]
    wv = w_in.rearrange("(kc p) n -> p kc n", p=128)
    with Stage(k) as S:
        hT = S.sb("hT", [128, 16, T], BF16); b_hT = bufs(NT)
        norm_to_hT(k, S, C, xs, b_xs, W["norm_mix_g"][l:l + 1, :], hT, b_hT, range(NT))
        ws = [S.sb("ws", [128, 16, 512], BF16) for _ in range(3)]; b_ws = bufs(3)
        pm = [S.ps("pm", [128, 512], F32) for _ in range(4)]; b_pm = bufs(4)
        sf32 = [S.sb("sf32", [128, T], F32) for _ in range(2)]; b_sf32 = bufs(2)
        sfb = [S.sb("sfb", [128, T], BF16) for _ in range(2)]; b_sfb = bufs(2)
        st32 = [S.sb("st32", [128, 512], F32) for _ in range(2)]; b_st32 = bufs(2)
        stb = [S.sb("stb", [128, 512], BF16) for _ in range(2)]; b_stb = bufs(2)
        wg = S.sb("wgate", [128, 16, 8], BF16); b_wg = Buf()
        cnt = {"w": 0, "p": 0, "f32": 0, "fb": 0, "t32": 0, "tb": 0}

        def load_w(c0, n):
            s = cnt["w"] % 3; cnt["w"] += 1
            k.dma("pool", ws[s][:, :, :n], wv[:, :, c0:c0 + n], writes=[b_ws[s]])
            return s

        def fm(s, off, M, dst, b_dst, dt, func=None):
            if dt == F32:
                i = cnt["f32"] % 2; cnt["f32"] += 1; stg, b_stg = sf32[i], b_sf32[i]
            else:
                i = cnt["fb"] % 2; cnt["fb"] += 1; stg, b_stg = sfb[i], b_sfb[i]
            for (t0, tn) in TBLK:
                p = cnt["p"] % 4; cnt["p"] += 1
                for kc in range(16):
                    lhsT = ws[s][:, kc, off:off + M] if s is not None else wg[:, kc, :]
                    k.op("pe", lambda e: e.matmul(pm[p][:M, :tn], lhsT=lhsT, rhs=hT[:, kc, t0:t0 + tn],
                                                  start=(kc == 0), stop=(kc == 15)),
                         reads=[b_ws[s] if s is not None else b_wg] + b_hT[t0 // 128:(t0 + tn) // 128], writes=[b_pm[p]])
                eng = "act" if (func is not None or p % 2 == 0) else "dve"
                evac(k, eng, stg[:M, t0:t0 + tn], pm[p][:M, :tn], [b_pm[p]], [b_stg], func=func)
            k.dma("sp", dst, stg[:M, :], reads=[b_stg], writes=[b_dst])

        def tm(s, off, n, dst, b_dst, dt, func=None):
            for t in range(NT):
                if dt == F32:
                    i = cnt["t32"] % 2; cnt["t32"] += 1; stg, b_stg = st32[i], b_st32[i]
                else:
                    i = cnt["tb"] % 2; cnt["tb"] += 1; stg, b_stg = stb[i], b_stb[i]
                p = cnt["p"] % 4; cnt["p"] += 1
                for kc in range(16):
                    k.op("pe", lambda e: e.matmul(pm[p][:, :n], lhsT=hT[:, kc, t * 128:(t + 1) * 128],
                                                  rhs=ws[s][:, kc, off:off + n], start=(kc == 0), stop=(kc == 15)),
                         reads=[b_ws[s], b_hT[t]], writes=[b_pm[p]])
                eng = "act" if (func is not None or p % 2 == 0) else "dve"
                evac(k, eng, stg[:, :n], pm[p][:, :n], [b_pm[p]], [b_stg], func=func)
                k.dma("sp", dst[t * 128:(t + 1) * 128, :], stg[:, :n], reads=[b_stg], writes=[b_dst])

        for cb in range(2):
            s = load_w(O_AQ + cb * 512, 512)
            for m in range(4):
                r0 = cb * 512 + m * 128
                fm(s, m * 128, 128, SC["qT"][r0:r0 + 128, :], SC["b_qT"], BF16)
        s = load_w(O_AK, 512)
        for m in range(2):
            fm(s, m * 128, 128, SC["kT"][m * 128:(m + 1) * 128, :], SC["b_kT"], BF16)
        tm(s, 256, 256, SC["v"], SC["b_v"], BF16)
        for cb in range(2):
            s = load_w(O_MQ + cb * 512, 512)
            for m in range(4):
                r0 = cb * 512 + m * 128
                fm(s, m * 128, 128, SC["mqk"][r0:r0 + 128, :], SC["b_mqk"], F32)
        for cb in range(2):
            s = load_w(O_MV + cb * 512, 512)
            tm(s, 0, 512, SC["mv"][:, cb * 512:(cb + 1) * 512], SC["b_mv"], BF16)
        k.dma("pool", wg[:], wv[:, :, O_MI:O_MI + 8], writes=[b_wg])
        fm(None, 0, 8, SC["gif"][:, :], SC["b_gif"], F32)
        for cb in range(2):
            s = load_w(O_MO + cb * 512, 512)
            tm(s, 0, 512, SC["smo"][:, cb * 512:(cb + 1) * 512], SC["b_smo"], F32, func=AF.Sigmoid)
        for name, off in (("sga", O_GA), ("sgm", O_GM)):
            for cb in range(4):
                s = load_w(off + cb * 512, 512)
                for m in range(4):
                    r0 = cb * 512 + m * 128
                    fm(s, m * 128, 128, SC[name][r0:r0 + 128, :], SC["b_" + name], F32, func=AF.Sigmoid)


def stage_attn(k, C, l, W, SC):
    with Stage(k) as S:
        q4 = [S.sb("q4", [64, 4, T], BF16) for _ in range(2)]; b_q4 = bufs(2)
        kj = [S.sb("kj", [64, T], BF16) for _ in range(2)]; b_kj = bufs(2)
        vj = [S.sb("vj", [128, NT, 65], BF16) for _ in range(2)]; b_vj = bufs(2)
        vm = [S.sb("vm", [16, 65], BF16) for _ in range(2)]; b_vm = bufs(2)
        oT = [S.sb("oT", [128, 2, T], BF16) for _ in range(2)]; b_oT = bufs(2)
        es = [S.sb("es", [128, 4], F32) for _ in range(2)]; b_es = bufs(2)
        ob = [S.sb("ob", [128, 4, 64], BF16) for _ in range(2)]; b_ob = bufs(2)
        for i in range(2):
            k.op("dve", lambda e: e.memset(vj[i][:, :, 64:65], 1.0), writes=[b_vj[i]])
            k.op("dve", lambda e: e.memset(vm[i][:, 64:65], 1.0), writes=[b_vm[i]])
        bpr = [[S.sb("bpr", [128, 4, 128], F32) for _ in range(2)] for _ in range(2)]
        bcu = [[S.sb("bcu", [128, 4, 128], F32) for _ in range(2)] for _ in range(2)]
        bme = [[S.sb("bme", [16, 4, 128], F32) for _ in range(2)] for _ in range(2)]
        b_bias = bufs(2)
        mpr = [S.sb("mpr", [128, 128], F32) for _ in range(2)]
        mcu = [S.sb("mcu", [128, 128], F32) for _ in range(2)]
        mme = [S.sb("mme", [16, 128], F32) for _ in range(2)]
        b_mask = Buf()
        for i in range(2):
            k.dma("sp", mpr[i][:], C["m_prev"][i], writes=[b_mask])
            k.dma("sp", mcu[i][:], C["m_cur"][i], writes=[b_mask])
            k.dma("sp", mme[i][:], C["m_meta"][i], writes=[b_mask])
        psA = [S.ps("psA", [128, 4, 128], F32) for _ in range(2)]; b_psA = bufs(2)
        psB = [S.ps("psB", [128, 4, 128], F32) for _ in range(2)]; b_psB = bufs(2)
        psC = [S.ps("psC", [16, 4, 128], F32) for _ in range(2)]; b_psC = bufs(2)
        psO = S.ps("psO", [128, 4, 128], F32); b_psO = Buf()
        ptr = S.ps("ptra", [128, 2, 128], BF16); b_ptr = Buf()
        sA = [S.sb("sA", [128, 4, 128], F32) for _ in range(2)]; b_sA = bufs(2)
        sB = [S.sb("sB", [128, 4, 128], F32) for _ in range(2)]; b_sB = bufs(2)
        sC = [S.sb("sC", [16, 4, 128], F32) for _ in range(2)]; b_sC = bufs(2)
        pA = [S.sb("pA", [128, 4, 128], BF16) for _ in range(3)]; b_pA = bufs(3)
        pB = [S.sb("pB", [128, 4, 128], BF16) for _ in range(3)]; b_pB = bufs(3)
        pC = [S.sb("pC", [16, 4, 128], BF16) for _ in range(3)]; b_pC = bufs(3)
        dn = [S.sb("dn", [128, 4], F32) for _ in range(2)]; b_dn = bufs(2)

        def load_group(j):
            s = j % 2
            k.dma("sp", q4[s][:], SC["qT"][j * 256:(j + 1) * 256, :].rearrange("(g d) t -> d g t", d=64),
                  reads=[SC["b_qT"]], writes=[b_q4[s]])
            k.dma("sp", kj[s][:], SC["kT"][j * 64:(j + 1) * 64, :], reads=[SC["b_kT"]], writes=[b_kj[s]])
            k.dma("sp", vj[s][:, :, 0:64], SC["v"][:, j * 64:(j + 1) * 64].rearrange("(n p) d -> p n d", p=128),
                  reads=[SC["b_v"]], writes=[b_vj[s]])
            k.dma("sp", vm[s][:, 0:64], SC["v"][112:128, j * 64:(j + 1) * 64], reads=[SC["b_v"]], writes=[b_vm[s]])
            k.dma("sp", es[s][:], W["attn_sinks"][l:l + 1, 4 * j:4 * j + 4].broadcast_to([128, 4]), writes=[b_es[s]])
            for i in range(2):
                for (bt, src, mk, P) in ((bpr[s][i], C["g_prev"][j], mpr[i], 128), (bcu[s][i], C["g_cur"][j], mcu[i], 128),
                                         (bme[s][i], C["g_meta"][j], mme[i], 16)):
                    k.dma("sp", bt[:], src, writes=[b_bias[s]])

        def prep_group(j):
            s = j % 2
            k.op("act", lambda e: e.activation(out=es[s][:], in_=es[s][:], func=AF.Exp), reads=[b_es[s]], writes=[b_es[s]])
            for i in range(2):
                for (bt, mk, P) in ((bpr[s][i], mpr[i], 128), (bcu[s][i], mcu[i], 128), (bme[s][i], mme[i], 16)):
                    k.op("pool", lambda e: e.tensor_tensor(out=bt[:], in0=bt[:], in1=mk[:].unsqueeze(1).broadcast_to([P, 4, 128]),
                                                           op=ALU.add), reads=[b_bias[s], b_mask], writes=[b_bias[s]])
                    k.op("act", lambda e: e.activation(out=bt[:], in_=bt[:], func=AF.Exp), reads=[b_bias[s]], writes=[b_bias[s]])

        def phase1(it, j, n):
            s = j % 2; u = it % 2; v = it % 3
            qn = q4[s][:, :, n * 128:(n + 1) * 128]
            rd = [b_q4[s], b_kj[s]]
            k.op("pe", lambda e: e.matmul(psB[u][:], lhsT=kj[s][:, n * 128:(n + 1) * 128], rhs=qn, start=True, stop=True),
                 reads=rd, writes=[b_psB[u]])
            k.op("act", lambda e: e.activation(out=sB[u][:], in_=psB[u][:], func=AF.Exp, scale=0.125), reads=[b_psB[u]], writes=[b_sB[u]])
            k.op("dve", lambda e: e.tensor_tensor(out=pB[v][:], in0=sB[u][:], in1=bcu[s][0 if n == 0 else 1][:], op=ALU.mult),
                 reads=[b_sB[u], b_bias[s]], writes=[b_pB[v]])
            if n >= 1:
                kk = 0 if n == 1 else 1
                k.op("pe", lambda e: e.matmul(psA[u][:], lhsT=kj[s][:, (n - 1) * 128:n * 128], rhs=qn, start=True, stop=True),
                     reads=rd, writes=[b_psA[u]])
                k.op("act", lambda e: e.activation(out=sA[u][:], in_=psA[u][:], func=AF.Exp, scale=0.125), reads=[b_psA[u]], writes=[b_sA[u]])
                k.op("pool", lambda e: e.tensor_tensor(out=pA[v][:], in0=sA[u][:], in1=bpr[s][kk][:], op=ALU.mult),
                     reads=[b_sA[u], b_bias[s]], writes=[b_pA[v]])
                k.op("pe", lambda e: e.matmul(psC[u][:], lhsT=kj[s][:, 112:128], rhs=qn, start=True, stop=True),
                     reads=rd, writes=[b_psC[u]])
                k.op("act", lambda e: e.activation(out=sC[u][:], in_=psC[u][:], func=AF.Exp, scale=0.125), reads=[b_psC[u]], writes=[b_sC[u]])
                k.op("pool", lambda e: e.tensor_tensor(out=pC[v][:], in0=sC[u][:], in1=bme[s][kk][:], op=ALU.mult),
                     reads=[b_sC[u], b_bias[s]], writes=[b_pC[v]])

        def phase2(it, j, n):
            s = j % 2; u = it % 2; v = it % 3
            for g in range(4):
                k.op("pe", lambda e: e.matmul(psO[:, g, 0:65], lhsT=pB[v][:, g, :], rhs=vj[s][:, n, :], start=True, stop=(n == 0)),
                     reads=[b_vj[s], b_pB[v]], writes=[b_psO])
                if n >= 1:
                    k.op("pe", lambda e: e.matmul(psO[:, g, 0:65], lhsT=pA[v][:, g, :], rhs=vj[s][:, n - 1, :], start=False, stop=False),
                         reads=[b_vj[s], b_pA[v]], writes=[b_psO])
                    k.op("pe", lambda e: e.matmul(psO[:, g, 0:65], lhsT=pC[v][:, g, :], rhs=vm[s][:], start=False, stop=True),
                         reads=[b_vm[s], b_pC[v]], writes=[b_psO])
            k.op("dve", lambda e: e.tensor_tensor(out=dn[u][:], in0=psO[:, :, 64], in1=es[s][:], op=ALU.add),
                 reads=[b_psO, b_es[s]], writes=[b_dn[u]])
            k.op("dve", lambda e: e.reciprocal(out=dn[u][:], in_=dn[u][:]), reads=[b_dn[u]], writes=[b_dn[u]])
            k.op("dve", lambda e: e.tensor_tensor(out=ob[u][:], in0=psO[:, :, 0:64],
                                                  in1=dn[u][:].unsqueeze(2).broadcast_to([128, 4, 64]), op=ALU.mult),
                 reads=[b_psO, b_dn[u]], writes=[b_ob[u]])
            obf = ob[u][:].rearrange("p g d -> p (g d)")
            for c in range(2):
                k.op("pe", lambda e: e.transpose(out=ptr[:, c, :], in_=obf[:, c * 128:(c + 1) * 128], identity=C["identb"][:]),
                     reads=[b_ob[u], C["b"]], writes=[b_ptr])
            evac(k, "act", oT[s][:, :, n * 128:(n + 1) * 128], ptr[:], [b_ptr], [b_oT[s]])
            if n == NT - 1:
                k.dma("sp", SC["attnT"][j * 256:(j + 1) * 256, :].rearrange("(c p) t -> p c t", p=128), oT[s][:],
                      reads=[b_oT[s]], writes=[SC["b_attnT"]])

        its = [(j, n) for j in range(4) for n in range(NT)]
        load_group(0)
        prep_group(0)
        for it, (j, n) in enumerate(its):
            phase1(it, j, n)
            if it >= 1:
                phase2(it - 1, *its[it - 1])
            if n == 2 and j + 1 < 4:
                load_group(j + 1)
            if n == 8 and j + 1 < 4:
                prep_group(j + 1)
        phase2(len(its) - 1, *its[-1])


def stage_mlstm(k, C, l, W, SC):
    with Stage(k) as S:
        tokq = S.sb("tokq", [128, NT, 12], F32); b_tokq = Buf()
        asc = S.sb("asc", [128, NT, 4], F32); b_asc = Buf()
        dec = S.sb("dec", [128, 4, NT], F32); b_dec = Buf()
        ngbc = S.sb("ngbc", [128, 1024], F32); b_ng = Buf()
        k.dma("sp", ngbc[:], W["mlstm_norm_g"][l:l + 1, :].broadcast_to([128, 1024]), writes=[b_ng])
        with Stage(k) as G:
            gi = G.sb("gi", [4, T], F32); gf = G.sb("gf", [4, T], F32); b_gi = Buf(); b_gf = Buf()
            ib = G.sb("ib", [4, 1], F32); fb = G.sb("fb", [4, 1], F32); b_ib = Buf(); b_fb = Buf()
            onesr = G.sb("onesr", [4, T], F32); b_or = Buf()
            Bn = G.sb("Bn", [4, T], F32); b_Bn = Buf()
            gg = G.sb("gg", [4, T], F32); b_gg = Buf()
            Mx = G.sb("Mx", [4, T], F32); b_Mx = Buf()
            Mp = G.sb("Mp", [4, NT], F32); b_Mp = Buf()
            tmp = G.sb("tmpg", [4, T], F32); b_tmp = Buf()
            qa = [G.sb("qa", [4, T], F32) for _ in range(3)]; b_qa = bufs(3)
            elast = G.sb("elast", [4, NT], F32); b_el = Buf()
            selh = G.sb("selh", [4, 4, 128], F32); b_selh = Buf()
            k.dma("sp", selh[:], C["selh"], writes=[b_selh])
            ptq = G.ps("ptq", [128, NT, 12], F32); b_ptq = Buf()
            pdec = G.ps("pdec", [128, 4, NT], F32); b_pdec = Buf()
            k.dma("sp", gi[:], SC["gif"][0:4, :], reads=[SC["b_gif"]], writes=[b_gi])
            k.dma("sp", gf[:], SC["gif"][4:8, :], reads=[SC["b_gif"]], writes=[b_gf])
            k.dma("sp", ib[:], W["igate_b"][l, :].rearrange("(h o) -> h o", o=1), writes=[b_ib])
            k.dma("sp", fb[:], W["fgate_b"][l, :].rearrange("(h o) -> h o", o=1), writes=[b_fb])
            k.op("dve", lambda e: e.memset(onesr[:], 1.0), writes=[b_or])
            k.op("dve", lambda e: e.tensor_scalar(out=fb[:], in0=fb[:], scalar1=-1.0, scalar2=None, op0=ALU.mult),
                 reads=[b_fb], writes=[b_fb])
            k.op("act", lambda e: e.activation(out=gf[:], in_=gf[:], func=AF.Exp, scale=-1.0, bias=fb[:]),
                 reads=[b_gf, b_fb], writes=[b_gf])
            k.op("act", lambda e: e.activation(out=gf[:], in_=gf[:], func=AF.Ln, scale=1.0, bias=1.0),
                 reads=[b_gf], writes=[b_gf])
            k.op("dve", lambda e: e.memset(gf[:, 0:112], 0.0), reads=[b_gf], writes=[b_gf])
            k.op("dve", lambda e: e.tensor_tensor_scan(out=Bn[:], data0=onesr[:], data1=gf[:], initial=0.0,
                                                       op0=ALU.mult, op1=ALU.add),
                 reads=[b_or, b_gf], writes=[b_Bn])
            k.op("dve", lambda e: e.scalar_tensor_tensor(out=gg[:], in0=gi[:], scalar=ib[:], in1=Bn[:],
                                                         op0=ALU.add, op1=ALU.add),
                 reads=[b_gi, b_ib, b_Bn], writes=[b_gg])
            k.op("dve", lambda e: e.memset(gg[:, 0:112], -1.0e30), reads=[b_gg], writes=[b_gg])
            k.op("dve", lambda e: e.tensor_tensor_scan(out=Mx[:], data0=onesr[:], data1=gg[:], initial=0.0,
                                                       op0=ALU.mult, op1=ALU.max),
                 reads=[b_or, b_gg], writes=[b_Mx])
            M3 = Mx[:].rearrange("p (n t) -> p n t", t=128)
            k.op("dve", lambda e: e.memset(Mp[:, 0:1], 0.0), writes=[b_Mp])
            k.op("dve", lambda e: e.tensor_copy(out=Mp[:, 1:NT], in_=M3[:, 0:NT - 1, 127]), reads=[b_Mx], writes=[b_Mp])
            Mpb = Mp[:].unsqueeze(2).broadcast_to([4, NT, 128])
            v3 = lambda t_: t_[:].rearrange("p (n t) -> p n t", t=128)
            k.op("dve", lambda e: e.tensor_tensor(out=v3(tmp), in0=v3(gg), in1=Mpb, op=ALU.subtract),
                 reads=[b_gg, b_Mp], writes=[b_tmp])
            k.op("act", lambda e: e.activation(out=qa[0][:], in_=tmp[:], func=AF.Exp), reads=[b_tmp], writes=[b_qa[0]])
            k.op("dve", lambda e: e.tensor_tensor(out=v3(tmp), in0=Mpb, in1=M3, op=ALU.subtract),
                 reads=[b_Mx, b_Mp, b_qa[0]], writes=[b_tmp])
            k.op("act", lambda e: e.activation(out=qa[1][:], in_=tmp[:], func=AF.Exp), reads=[b_tmp], writes=[b_qa[1]])
            k.op("dve", lambda e: e.tensor_tensor(out=tmp[:], in0=Bn[:], in1=Mx[:], op=ALU.subtract),
                 reads=[b_Bn, b_Mx, b_qa[1]], writes=[b_tmp])
            k.op("act", lambda e: e.activation(out=qa[2][:], in_=tmp[:], func=AF.Exp), reads=[b_tmp], writes=[b_qa[2]])
            for qi in range(3):
                for n in range(NT):
                    k.op("pe", lambda e: e.transpose(out=ptq[:, n, qi * 4:(qi + 1) * 4], in_=qa[qi][:, n * 128:(n + 1) * 128],
                                                     identity=C["identf"][0:4, 0:4]),
                         reads=[b_qa[qi], C["b"]], writes=[b_ptq])
            evac(k, "dve", tokq[:], ptq[:], [b_ptq], [b_tokq])
            k.op("dve", lambda e: e.tensor_scalar(out=asc[:], in0=tokq[:, :, 0:4], scalar1=128.0 ** -0.5, scalar2=None,
                                                  op0=ALU.mult), reads=[b_tokq], writes=[b_asc])
            k.op("dve", lambda e: e.tensor_copy(out=elast[:], in_=v3(qa[1])[:, :, 127]), reads=[b_qa[1]], writes=[b_el])
            for h in range(4):
                k.op("pe", lambda e: e.matmul(pdec[:, h, :], lhsT=selh[:, h, :], rhs=elast[:], start=True, stop=True),
                     reads=[b_el, b_selh], writes=[b_pdec])
            evac(k, "dve", dec[:], pdec[:], [b_pdec], [b_dec])
        qT = S.sb("mqT", [128, 4, T], BF16); b_qT = Buf()
        kT = S.sb("mkT", [128, 4, T], BF16); b_kT = Buf()
        ktok = S.sb("ktok", [128, NT, 512], BF16); b_ktok = Buf()
        with Stage(k) as V:
            cwb = V.sb("cwb", [128, 8, 5], F32); b_cw = Buf()
            k.dma("sp", cwb[:], W["conv_pk"][l], writes=[b_cw])
            xin = [V.sb("xin", [128, 3 + T], F32) for _ in range(2)]; b_xin = bufs(2)
            acc = [V.sb("acc", [128, T], F32) for _ in range(2)]; b_acc = bufs(2)
            pk = [V.ps("pk", [128, 4, 128], BF16) for _ in range(2)]; b_pk = bufs(2)
            for s in range(2):
                k.op("dve", lambda e: e.memset(xin[s][:, 0:3], 0.0), writes=[b_xin[s]])
            for c in range(8):
                s = c % 2
                k.dma("sp", xin[s][:, 3:3 + T], SC["mqk"][c * 128:(c + 1) * 128, :], reads=[SC["b_mqk"]], writes=[b_xin[s]])
                k.op("dve", lambda e: e.tensor_scalar(out=acc[s][:], in0=xin[s][:, 3:3 + T], scalar1=cwb[:, c, 3:4],
                                                      scalar2=cwb[:, c, 4:5], op0=ALU.mult, op1=ALU.add),
                     reads=[b_xin[s], b_cw], writes=[b_acc[s]])
                for j in range(3):
                    k.op("dve", lambda e: e.scalar_tensor_tensor(out=acc[s][:], in0=xin[s][:, j:j + T], scalar=cwb[:, c, j:j + 1],
                                                                 in1=acc[s][:], op0=ALU.mult, op1=ALU.add),
                         reads=[b_xin[s], b_cw, b_acc[s]], writes=[b_acc[s]])
                dst, b_dst = (qT, b_qT) if c < 4 else (kT, b_kT)
                k.op("act", lambda e: e.activation(out=dst[:, c % 4, :], in_=acc[s][:], func=AF.Silu),
                     reads=[b_acc[s]], writes=[b_dst])
            for n in range(NT):
                u = n % 2
                for h in range(4):
                    k.op("pe", lambda e: e.transpose(out=pk[u][:, h, :], in_=kT[:, h, n * 128:(n + 1) * 128], identity=C["identb"][:]),
                         reads=[b_kT, C["b"]], writes=[b_pk[u]])
                evac(k, "act" if u == 0 else "dve", ktok[:, n, :], pk[u][:].rearrange("p h t -> p (h t)"), [b_pk[u]], [b_ktok])
        vp = S.sb("vp", [128, NT, 4, 258], BF16); b_vp = bufs(NT)
        with Stage(k) as V2:
            mvt = V2.sb("mvt", [128, NT, 1024], BF16); b_mvt = Buf()
            k.dma("sp", mvt[:], SC["mv"].rearrange("(n p) c -> p n c", p=128), reads=[SC["b_mv"]], writes=[b_mvt])
            for n in range(NT):
                for h in range(4):
                    k.op("dve" if h % 2 == 0 else "pool",
                         lambda e: e.tensor_scalar(out=vp[:, n, h, 0:256], in0=mvt[:, n, h * 256:(h + 1) * 256],
                                                   scalar1=asc[:, n, h:h + 1], scalar2=None, op0=ALU.mult),
                         reads=[b_mvt, b_asc], writes=[b_vp[n]])
                k.op("dve", lambda e: e.tensor_copy(out=vp[:, n, :, 256:257], in_=asc[:, n, :].unsqueeze(2)),
                     reads=[b_asc], writes=[b_vp[n]])
        mT = S.sb("mT", [128, 8, T], BF16); b_mT = Buf()
        CT32 = S.sb("CT32", [128, 4, 256], F32); b_C32 = Buf()
        CTb = S.sb("CTb", [128, 4, 256], BF16); b_Cb = Buf()
        n32 = S.sb("n32", [128, 4], F32); b_n32 = Buf()
        nb = S.sb("nb", [128, 4], BF16); b_nb = Buf()
        tmpC = S.sb("tmpC", [128, 4, 256], F32); b_tmpC = Buf()
        tmpn = S.sb("tmpn", [128, 4], F32); b_tmpn = Buf()
        AT = [S.sb("AT", [128, 4, 128], BF16) for _ in range(2)]; b_AT = bufs(2)
        Gt = [S.sb("Gt", [128, 1024], F32) for _ in range(2)]; b_Gt = bufs(2)
        mt = [S.sb("mt", [128, 1024], BF16) for _ in range(2)]; b_mt = bufs(2)
        junk = S.sb("junkm", [128, 256], BF16); b_junk = Buf()
        t1 = [S.sb("t1", [128, 4], F32) for _ in range(2)]; b_t1 = bufs(2)
        r4 = [S.sb("r4", [128, 4], F32) for _ in range(2)]; b_r4 = bufs(2)
        ss4 = [S.sb("ss4", [128, 4], F32) for _ in range(2)]; b_ss4 = bufs(2)
        psA = S.ps("mpsA", [128, 4, 128], F32); b_psA = Buf()
        psO = S.ps("mpsO", [128, 4, 256], F32); b_psO = Buf()
        psC = S.ps("mpsC", [128, 4, 256], F32); b_psC = Buf()
        psD = S.ps("mpsD", [128, 8], F32); b_psD = Buf(); b_psN = Buf()
        ptm = S.ps("mptm", [128, 8, 128], BF16); b_ptm = Buf()
        k.op("dve", lambda e: e.memset(CT32[:], 0.0), writes=[b_C32])
        k.op("dve", lambda e: e.memset(CTb[:], 0.0), writes=[b_Cb])
        k.op("dve", lambda e: e.memset(n32[:], 0.0), writes=[b_n32])
        k.op("dve", lambda e: e.memset(nb[:], 0.0), writes=[b_nb])
        tri = C["tri"]
        for n in range(NT):
            u = n % 2
            tok = slice(n * 128, (n + 1) * 128)
            k.dma("sp", Gt[u][:], SC["smo"][tok, :], reads=[SC["b_smo"]], writes=[b_Gt[u]])
            k.op("pool", lambda e: e.tensor_tensor(out=Gt[u][:], in0=Gt[u][:], in1=ngbc[:], op=ALU.mult),
                 reads=[b_Gt[u], b_ng], writes=[b_Gt[u]])
            for h in range(4):
                k.op("pe", lambda e: e.matmul(psA[:, h, :], lhsT=kT[:, h, tok], rhs=qT[:, h, tok], start=True, stop=True),
                     reads=[b_kT, b_qT], writes=[b_psA])
            k.op("dve", lambda e: e.tensor_tensor(out=AT[u][:], in0=psA[:], in1=tri[:].unsqueeze(1).broadcast_to([128, 4, 128]),
                                                  op=ALU.mult),
                 reads=[b_psA, C["b"]], writes=[b_AT[u]])
            for h in range(4):
                k.op("pe", lambda e: e.matmul(psO[:, h, :], lhsT=AT[u][:, h, :], rhs=vp[:, n, h, 0:256], start=True, stop=False),
                     reads=[b_AT[u], b_vp[n]], writes=[b_psO])
                k.op("pe", lambda e: e.matmul(psO[:, h, :], lhsT=qT[:, h, tok], rhs=CTb[:, h, :], start=False, stop=True),
                     reads=[b_qT, b_Cb], writes=[b_psO])
                k.op("pe", lambda e: e.matmul(psD[:, h:h + 1], lhsT=AT[u][:, h, :], rhs=vp[:, n, h, 256:257], start=True, stop=False),
                     reads=[b_AT[u], b_vp[n]], writes=[b_psD])
                k.op("pe", lambda e: e.matmul(psD[:, h:h + 1], lhsT=qT[:, h, tok], rhs=nb[:, h:h + 1], start=False, stop=True),
                     reads=[b_qT, b_nb], writes=[b_psD])
            if n < NT - 1:
                for h in range(4):
                    k.op("pe", lambda e: e.matmul(psC[:, h, :], lhsT=ktok[:, n, h * 128:(h + 1) * 128], rhs=vp[:, n, h, 0:256],
                                                  start=True, stop=True),
                         reads=[b_ktok, b_vp[n]], writes=[b_psC])
                    k.op("pe", lambda e: e.matmul(psD[:, 4 + h:5 + h], lhsT=ktok[:, n, h * 128:(h + 1) * 128],
                                                  rhs=vp[:, n, h, 256:257], start=True, stop=True),
                         reads=[b_ktok, b_vp[n]], writes=[b_psN])
                k.op("dve", lambda e: e.tensor_tensor(out=tmpC[:], in0=psC[:], in1=CT32[:], op=ALU.add),
                     reads=[b_psC, b_C32], writes=[b_tmpC])
                k.op("dve", lambda e: e.tensor_tensor(out=CT32[:], in0=tmpC[:],
                                                      in1=dec[:, :, n].unsqueeze(2).broadcast_to([128, 4, 256]), op=ALU.mult),
                     reads=[b_tmpC, b_dec], writes=[b_C32])
                k.op("act", lambda e: e.activation(out=CTb[:], in_=CT32[:], func=AF.Copy), reads=[b_C32], writes=[b_Cb])
                k.op("dve", lambda e: e.tensor_tensor(out=tmpn[:], in0=psD[:, 4:8], in1=n32[:], op=ALU.add),
                     reads=[b_psN, b_n32], writes=[b_tmpn])
                k.op("dve", lambda e: e.tensor_tensor(out=n32[:], in0=tmpn[:], in1=dec[:, :, n], op=ALU.mult),
                     reads=[b_tmpn, b_dec], writes=[b_n32])
                k.op("act", lambda e: e.activation(out=nb[:], in_=n32[:], func=AF.Copy), reads=[b_n32], writes=[b_nb])
            k.op("dve", lambda e: e.tensor_tensor(out=t1[u][:], in0=psD[:, 0:4], in1=tokq[:, n, 4:8], op=ALU.mult),
                 reads=[b_psD, b_tokq], writes=[b_t1[u]])
            k.op("act", lambda e: e.activation(out=t1[u][:], in_=t1[u][:], func=AF.Abs),
                 reads=[b_t1[u]], writes=[b_t1[u]])
            k.op("dve", lambda e: e.tensor_tensor(out=t1[u][:], in0=t1[u][:], in1=tokq[:, n, 8:12], op=ALU.max),
                 reads=[b_t1[u], b_tokq], writes=[b_t1[u]])
            k.op("dve", lambda e: e.reciprocal(out=t1[u][:], in_=t1[u][:]), reads=[b_t1[u]], writes=[b_t1[u]])
            k.op("dve", lambda e: e.tensor_tensor(out=r4[u][:], in0=t1[u][:], in1=tokq[:, n, 4:8], op=ALU.mult),
                 reads=[b_t1[u], b_tokq], writes=[b_r4[u]])
            for h in range(4):
                k.op("act", lambda e: e.activation(out=junk[:], in_=psO[:, h, :], func=AF.Square, scale=r4[u][:, h:h + 1],
                                                   accum_out=ss4[u][:, h:h + 1]),
                     reads=[b_psO, b_r4[u]], writes=[b_junk, b_ss4[u]])
            k.op("act", lambda e: e.activation(out=ss4[u][:], in_=ss4[u][:], func=AF.Sqrt, scale=1.0 / 256, bias=EPS),
                 reads=[b_ss4[u]], writes=[b_ss4[u]])
            k.op("dve", lambda e: e.reciprocal(out=ss4[u][:], in_=ss4[u][:]), reads=[b_ss4[u]], writes=[b_ss4[u]])
            k.op("dve", lambda e: e.tensor_tensor(out=r4[u][:], in0=r4[u][:], in1=ss4[u][:], op=ALU.mult),
                 reads=[b_r4[u], b_ss4[u]], writes=[b_r4[u]])
            for h in range(4):
                k.op("dve", lambda e: e.scalar_tensor_tensor(out=mt[u][:, h * 256:(h + 1) * 256], in0=psO[:, h, :],
                                                             scalar=r4[u][:, h:h + 1], in1=Gt[u][:, h * 256:(h + 1) * 256],
                                                             op0=ALU.mult, op1=ALU.mult),
                     reads=[b_psO, b_r4[u], b_Gt[u]], writes=[b_mt[u]])
            for c in range(8):
                k.op("pe", lambda e: e.transpose(out=ptm[:, c, :], in_=mt[u][:, c * 128:(c + 1) * 128], identity=C["identb"][:]),
                     reads=[b_mt[u], C["b"]], writes=[b_ptm])
            evac(k, "act", mT[:, :, tok], ptm[:], [b_ptm], [b_mT])
        k.dma("sp", SC["mlT"].rearrange("(c p) t -> p c t", p=128), mT[:], reads=[b_mT], writes=[SC["b_mlT"]])


def t5_bucket_np(rel):
    n = np.maximum(rel, 0)
    large = 16 + (np.log(np.maximum(n, 1).astype(np.float32) / np.float32(16)) / np.float32(np.log(8.0))
                  * np.float32(16)).astype(np.int32)
    large = np.minimum(large, 31)
    return np.where(n < 16, n, large)


def pack_conv(conv_w, conv_b):
    out = np.zeros((DEPTH, 128, 8, 5), np.float32)
    for l in range(DEPTH):
        out[l, :, :, 0:4] = conv_w[l].T.reshape(8, 128, 4).transpose(1, 0, 2)
        out[l, :, :, 4] = conv_b[l].reshape(8, 128).T
    return out


def make_consts():
    c = {}
    c["identb"] = np.eye(128, dtype=np.float32).astype(ml_dtypes.bfloat16)
    c["identf"] = np.eye(128, dtype=np.float32)
    kk = np.arange(128)[:, None]
    qq = np.arange(128)[None, :]
    c["tri"] = (kk <= qq).astype(np.float32)
    selh = np.zeros((4, 4, 128), np.float32)
    for h in range(4):
        selh[h, h, :] = 1.0
    c["selh"] = selh
    mp = np.zeros((2, 128, 128), np.float32)
    mp[1] = np.where(kk > qq, 0.0, NEG)
    mp[0] = np.where((kk > qq) & (kk >= 112), 0.0, NEG)
    mc = np.zeros((2, 128, 128), np.float32)
    mc[1] = np.where(kk <= qq, 0.0, NEG)
    mc[0] = np.where((kk <= qq) & (kk >= 112), 0.0, NEG)
    mm = np.zeros((2, 16, 128), np.float32)
    m16 = np.arange(16)[:, None]
    mm[0] = np.where(qq - m16 >= 112, 0.0, NEG)
    c["m_prev"], c["m_cur"], c["m_meta"] = mp, mc, mm
    c["iota"] = np.arange(CAP, dtype=np.float32).reshape(1, CAP)
    c["trib"] = c["tri"].astype(ml_dtypes.bfloat16)
    return c


def make_gathers(table):
    kk = np.arange(128)[:, None]
    qq = np.arange(128)[None, :]
    b_prev = t5_bucket_np(qq + 128 - kk)
    b_cur = t5_bucket_np(qq - kk)
    gp = np.zeros((4, 128, 4, 128), np.float32)
    gc = np.zeros((4, 128, 4, 128), np.float32)
    gm = np.zeros((4, 16, 4, 128), np.float32)
    for j in range(4):
        for g in range(4):
            h = 4 * j + g
            gp[j, :, g, :] = table[b_prev, h]
            gc[j, :, g, :] = table[b_cur, h]
            gm[j, :, g, :] = table[31, h]
    return gp, gc, gm


CONST_SHAPES = {
    "identb": ([128, 128], BF16), "identf": ([128, 128], F32), "tri": ([128, 128], F32), "selh": ([4, 4, 128], F32),
    "m_prev": ([2, 128, 128], F32), "m_cur": ([2, 128, 128], F32), "m_meta": ([2, 16, 128], F32),
    "iota": ([1, CAP], F32), "trib": ([128, 128], BF16),
    "g_prev": ([4, 128, 4, 128], F32), "g_cur": ([4, 128, 4, 128], F32), "g_meta": ([4, 16, 4, 128], F32),
}
W_SHAPES = {
    "w_in": [DEPTH, D, 8712], "attn_sinks": [DEPTH, 16], "conv_pk": [DEPTH, 128, 8, 5], "igate_b": [DEPTH, 4],
    "fgate_b": [DEPTH, 4], "mlstm_norm_g": [DEPTH, 1024], "w_attn_up": [DEPTH, 1024, D], "w_mlstm_up": [DEPTH, 1024, D],
    "w_out": [DEPTH, D, D], "norm_mix_g": [DEPTH, D], "norm_ffn_g": [DEPTH, D], "w_ffn_gate": [1, D, DFF],
    "w_ffn_up": [1, D, DFF], "w_ffn_down": [1, DFF, D], "w_router": [1, D, NE], "b_router": [1, NE],
    "w_moe_gate": [1, NE, D, DFF], "w_moe_up": [1, NE, D, DFF], "w_moe_down": [1, NE, DFF, D], "final_norm_g": [1, D],
}
SC_SHAPES = {
    "xs": ([T, D], F32), "qT": ([1024, T], BF16), "kT": ([256, T], BF16), "v": ([T, 256], BF16),
    "mqk": ([1024, T], F32), "mv": ([T, 1024], BF16), "gif": ([8, T], F32), "smo": ([T, 1024], F32),
    "sga": ([D, T], F32), "sgm": ([D, T], F32), "attnT": ([1024, T], BF16), "mlT": ([1024, T], BF16),
    "h2b": ([16, 128, 16, 128], BF16),
}


def build(stages, feed=(), expose=(), used_w=None):
    nc = bass.Bass("TRN2", target_bir_lowering=False)
    W = {}
    for name, shp in W_SHAPES.items():
        if used_w is not None and name not in used_w:
            continue
        W[name] = nc.dram_tensor(name, shp, F32, kind="ExternalInput").ap()
    x = nc.dram_tensor("x", [SEQ, D], F32, kind="ExternalInput").ap()
    meta = nc.dram_tensor("meta", [16, D], F32, kind="ExternalInput").ap()
    CD = {n: nc.dram_tensor("c_" + n, shp, dt, kind="ExternalInput").ap() for n, (shp, dt) in CONST_SHAPES.items()}
    out = nc.dram_tensor("out", [SEQ, D], F32, kind="ExternalOutput").ap()
    dbg = nc.dram_tensor("dbg_mask", [128, 16, 8], F32, kind="ExternalOutput").ap()
    SC = {}
    for n, (shp, dt) in SC_SHAPES.items():
        kind = "ExternalInput" if n in feed else ("ExternalOutput" if n in expose else "Internal")
        SC[n] = nc.dram_tensor("s_" + n, shp, dt, kind=kind).ap()
        SC["b_" + n] = Buf()
    b_xs = bufs(NT)
    with ExitStack() as st:
        k = K(nc, st)
        C = {"b": Buf()}
        for n in ("identb", "identf", "tri"):
            shp, dt = CONST_SHAPES[n]
            C[n] = st.enter_context(nc.sbuf_tensor("C_" + n, shp, dt))
            k.dma("sp", C[n][:], CD[n], writes=[C["b"]])
        for n in ("m_prev", "m_cur", "m_meta", "g_prev", "g_cur", "g_meta", "iota", "trib", "selh"):
            C[n] = CD[n]
        for stg in stages:
            if stg == "init":
                stage_init(k, C, x, meta, SC["xs"], b_xs)
            elif stg[0] == "A":
                stage_A(k, C, int(stg[1]), W, SC["xs"], b_xs, SC)
            elif stg.startswith("attn"):
                stage_attn(k, C, int(stg[4]), W, SC)
            elif stg.startswith("ml"):
                stage_mlstm(k, C, int(stg[2]), W, SC)
            elif stg[0] == "C":
                stage_C(k, C, int(stg[1]), W, SC["xs"], b_xs, SC)
            elif stg.startswith("ffn"):
                stage_ffn_dense(k, C, int(stg[3]), W, SC["xs"], b_xs)
            elif stg == "moe":
                stage_moe(k, C, 1, W, SC["xs"], b_xs, SC, dbg, out if "final" not in stages else None)
            elif stg == "final":
                stage_final(k, C, W, SC["xs"], b_xs, out)
            else:
                raise ValueError(stg)
        k.barrier()
        print("n_inst", k.n_inst, {e: k.ecnt[e] for e in k.ecnt})
    return nc


def stage_init(k, C, x, meta, xs, b_xs):
    with Stage(k) as S:
        z = S.sb("z0", [112, D], F32); bz = Buf()
        k.op("dve", lambda e: e.memset(z[:], 0.0), writes=[bz])
        k.dma("sp", xs[0:112, :], z[:], reads=[bz], writes=[b_xs[0]])
        k.dma("sp", xs[112:128, :], meta, writes=[b_xs[0]])
        for t in range(1, NT):
            k.dma("sp", xs[t * 128:(t + 1) * 128, :], x[(t - 1) * 128:t * 128, :], writes=[b_xs[t]])


def stage_C(k, C, l, W, xs, b_xs, SC):
    with Stage(k) as S:
        yT = S.sb("yT", [128, 16, T], BF16); b_yT = Buf()
        with Stage(k) as S1:
            aT = S1.sb("aT", [128, 8, T], BF16); b_aT = Buf()
            mT = S1.sb("mT2", [128, 8, T], BF16); b_mT = Buf()
            k.dma("sp", aT[:], SC["attnT"].rearrange("(c p) t -> p c t", p=128), reads=[SC["b_attnT"]], writes=[b_aT])
            k.dma("sp", mT[:], SC["mlT"].rearrange("(c p) t -> p c t", p=128), reads=[SC["b_mlT"]], writes=[b_mT])
            wa = [S1.sb("wa", [128, 8, 512], BF16) for _ in range(2)]; b_wa = bufs(2)
            wm = [S1.sb("wm", [128, 8, 512], BF16) for _ in range(2)]; b_wm = bufs(2)
            ga = [S1.sb("ga", [128, 512], F32) for _ in range(4)]; b_ga = bufs(4)
            gm = [S1.sb("gm", [128, 512], F32) for _ in range(4)]; b_gm = bufs(4)
            ta = [S1.sb("ta", [128, 512], F32) for _ in range(2)]; b_ta = bufs(2)
            tb = [S1.sb("tb", [128, 512], F32) for _ in range(2)]; b_tb = bufs(2)
            pa = [S1.ps("pa", [128, 512], F32) for _ in range(2)]; b_pa = bufs(2)
            pb = [S1.ps("pb", [128, 512], F32) for _ in range(2)]; b_pb = bufs(2)
            wav = W["w_attn_up"][l].rearrange("(kc p) n -> p kc n", p=128)
            wmv = W["w_mlstm_up"][l].rearrange("(kc p) n -> p kc n", p=128)
            it = 0
            for fb in range(4):
                s = fb % 2
                k.dma("pool", wa[s][:], wav[:, :, fb * 512:(fb + 1) * 512], writes=[b_wa[s]])
                k.dma("pool", wm[s][:], wmv[:, :, fb * 512:(fb + 1) * 512], writes=[b_wm[s]])
                for m in range(4):
                    f = fb * 4 + m
                    for (t0, tn) in TBLK:
                        u = it % 2; g4 = it % 4; it += 1
                        k.dma("sp", ga[g4][:, :tn], SC["sga"][f * 128:(f + 1) * 128, t0:t0 + tn], reads=[SC["b_sga"]], writes=[b_ga[g4]])
                        k.dma("sp", gm[g4][:, :tn], SC["sgm"][f * 128:(f + 1) * 128, t0:t0 + tn], reads=[SC["b_sgm"]], writes=[b_gm[g4]])
                        for kc in range(8):
                            k.op("pe", lambda e: e.matmul(pa[u][:, :tn], lhsT=wa[s][:, kc, m * 128:(m + 1) * 128],
                                                          rhs=aT[:, kc, t0:t0 + tn], start=(kc == 0), stop=(kc == 7)),
                                 reads=[b_wa[s], b_aT], writes=[b_pa[u]])
                        for kc in range(8):
                            k.op("pe", lambda e: e.matmul(pb[u][:, :tn], lhsT=wm[s][:, kc, m * 128:(m + 1) * 128],
                                                          rhs=mT[:, kc, t0:t0 + tn], start=(kc == 0), stop=(kc == 7)),
                                 reads=[b_wm[s], b_mT], writes=[b_pb[u]])
                        k.op("dve", lambda e: e.tensor_tensor(out=ta[u][:, :tn], in0=pa[u][:, :tn], in1=ga[g4][:, :tn], op=ALU.mult),
                             reads=[b_pa[u], b_ga[g4]], writes=[b_ta[u]])
                        k.op("dve", lambda e: e.tensor_tensor(out=tb[u][:, :tn], in0=pb[u][:, :tn], in1=gm[g4][:, :tn], op=ALU.mult),
                             reads=[b_pb[u], b_gm[g4]], writes=[b_tb[u]])
                        k.op("pool", lambda e: e.tensor_tensor(out=yT[:, f, t0:t0 + tn], in0=ta[u][:, :tn], in1=tb[u][:, :tn], op=ALU.add),
                             reads=[b_ta[u], b_tb[u]], writes=[b_yT])
        with Stage(k) as S2:
            wo = [S2.sb("wo", [128, 16, 512], BF16) for _ in range(2)]; b_wo = bufs(2)
            xt = [S2.sb("xtc", [128, 512], F32) for _ in range(3)]; b_xt = bufs(3)
            po = [S2.ps("po", [128, 512], F32) for _ in range(3)]; b_po = bufs(3)
            wov = W["w_out"][l].rearrange("(kc p) n -> p kc n", p=128)
            it = 0
            for cb in range(4):
                s = cb % 2
                k.dma("pool", wo[s][:], wov[:, :, cb * 512:(cb + 1) * 512], writes=[b_wo[s]])
                for t in range(NT):
                    u = it % 3; it += 1
                    rows = slice(t * 128, (t + 1) * 128)
                    cols = slice(cb * 512, (cb + 1) * 512)
                    k.dma("sp", xt[u][:], xs[rows, cols], reads=[b_xs[t]], writes=[b_xt[u]])
                    for kc in range(16):
                        k.op("pe", lambda e: e.matmul(po[u][:], lhsT=yT[:, kc, rows], rhs=wo[s][:, kc, :],
                                                      start=(kc == 0), stop=(kc == 15)),
                             reads=[b_yT, b_wo[s]], writes=[b_po[u]])
                    k.op("dve", lambda e: e.tensor_tensor(out=xt[u][:], in0=po[u][:], in1=xt[u][:], op=ALU.add),
                         reads=[b_po[u], b_xt[u]], writes=[b_xt[u]])
                    k.dma("act", xs[rows, cols], xt[u][:], reads=[b_xt[u]], writes=[b_xs[t]])


def ffn_alloc(S, N):
    nsub = -(-N // 512)
    step = -(-N // nsub)
    NW = 3
    R = {"NW": NW, "step": step}
    R["wg"] = [S.sb("wg", [128, 16, 256], BF16) for _ in range(NW)]; R["b_wg"] = bufs(NW)
    R["wu"] = [S.sb("wu", [128, 16, 256], BF16) for _ in range(NW)]; R["b_wu"] = bufs(NW)
    R["wd"] = [S.sb("wd", [128, NFF, 256], BF16) for _ in range(2)]; R["b_wd"] = bufs(2)
    R["sg"] = [S.sb("sg", [128, step], F32) for _ in range(2)]; R["b_sg"] = bufs(2)
    R["psG"] = [S.ps("psG", [128, 512], F32) for _ in range(2)]; R["b_psG"] = bufs(2)
    R["psU"] = [S.ps("psU", [128, 512], F32) for _ in range(2)]; R["b_psU"] = bufs(2)
    R["psY"] = [S.ps("psY", [128, 512], F32) for _ in range(2)]; R["b_psY"] = bufs(2)
    R["cnt"] = [0, 0, 0]
    return R


def ffn_block(k, R, xT, b_xT, N, Wg, Wu, Wd, act, b_act, epilogue):
    step = R["step"]
    subs = [(i, min(step, N - i)) for i in range(0, N, step)]
    tiles = [(i, min(128, N - i)) for i in range(0, N, 128)]
    NW = R["NW"]
    wg, wu, wd, sg, psG, psU, psY = R["wg"], R["wu"], R["wd"], R["sg"], R["psG"], R["psU"], R["psY"]
    b_wg, b_wu, b_wd, b_sg, b_psG, b_psU, b_psY = R["b_wg"], R["b_wu"], R["b_wd"], R["b_sg"], R["b_psG"], R["b_psU"], R["b_psY"]
    cnt = R["cnt"]
    Wgv = Wg.rearrange("(kc p) n -> p kc n", p=128)
    Wuv = Wu.rearrange("(kc p) n -> p kc n", p=128)
    Wdv = Wd.rearrange("(fc p) n -> p fc n", p=128)
    for fg in range(NFF // 2):
        s = cnt[0] % NW; cnt[0] += 1
        k.dma("pool", wg[s][:], Wgv[:, :, fg * 256:(fg + 1) * 256], writes=[b_wg[s]])
        k.dma("pool", wu[s][:], Wuv[:, :, fg * 256:(fg + 1) * 256], writes=[b_wu[s]])
        for c in range(2):
            ffc = fg * 2 + c
            for (t0, tn) in subs:
                u = cnt[1] % 2; cnt[1] += 1
                for kc in range(16):
                    k.op("pe", lambda e: e.matmul(psG[u][:, :tn], lhsT=wg[s][:, kc, c * 128:(c + 1) * 128],
                                                  rhs=xT[:, kc, t0:t0 + tn], start=(kc == 0), stop=(kc == 15)),
                         reads=[b_wg[s], b_xT], writes=[b_psG[u]])
                for kc in range(16):
                    k.op("pe", lambda e: e.matmul(psU[u][:, :tn], lhsT=wu[s][:, kc, c * 128:(c + 1) * 128],
                                                  rhs=xT[:, kc, t0:t0 + tn], start=(kc == 0), stop=(kc == 15)),
                         reads=[b_wu[s], b_xT], writes=[b_psU[u]])
                k.op("act", lambda e: e.activation(out=sg[u][:, :tn], in_=psG[u][:, :tn], func=AF.Silu),
                     reads=[b_psG[u]], writes=[b_sg[u]])
                k.op("dve", lambda e: e.tensor_tensor(out=act[:, ffc, t0:t0 + tn], in0=sg[u][:, :tn], in1=psU[u][:, :tn], op=ALU.mult),
                     reads=[b_sg[u], b_psU[u]], writes=b_act)
    for cb in range(8):
        s = cnt[2] % 2; cnt[2] += 1
        k.dma("pool", wd[s][:], Wdv[:, :, cb * 256:(cb + 1) * 256], writes=[b_wd[s]])
        for ti, (s0, sn) in enumerate(tiles):
            u = cnt[1] % 2; cnt[1] += 1
            for ffc in range(NFF):
                k.op("pe", lambda e: e.matmul(psY[u][:sn, :256], lhsT=act[:, ffc, s0:s0 + sn], rhs=wd[s][:, ffc, :],
                                              start=(ffc == 0), stop=(ffc == NFF - 1)),
                     reads=b_act + [b_wd[s]], writes=[b_psY[u]])
            epilogue(ti, cb, psY[u][:sn, :256], b_psY[u])


def stage_ffn_dense(k, C, l, W, xs, b_xs):
    blocks = [(0, 6), (6, 6), (12, 5)]
    for (tile0, ntl) in blocks:
        N = ntl * 128
        with Stage(k) as S:
            xT = S.sb("fxT", [128, 16, N], BF16); b_xT_l = bufs(ntl)
            act = S.sb("fact", [128, NFF, N], BF16); b_act = Buf()
            with Stage(k) as S0:
                norm_to_hT(k, S0, C, xs, b_xs, W["norm_ffn_g"][l:l + 1, :], xT, b_xT_l, range(tile0, tile0 + ntl), tok0=tile0)
            b_xT = Buf()
            with Stage(k) as S1:
                xr = [S1.sb("xr", [128, 256], F32) for _ in range(3)]; b_xr = bufs(3)
                cnt = [0]

                def epi(ti, cb, ps, b_ps):
                    u = cnt[0] % 3; cnt[0] += 1
                    t = tile0 + ti
                    rows = slice(t * 128, (t + 1) * 128); cols = slice(cb * 256, (cb + 1) * 256)
                    k.dma("sp", xr[u][:], xs[rows, cols], reads=[b_xs[t]], writes=[b_xr[u]])
                    k.op("dve", lambda e: e.tensor_tensor(out=xr[u][:], in0=ps, in1=xr[u][:], op=ALU.add),
                         reads=[b_ps, b_xr[u]], writes=[b_xr[u]])
                    k.dma("act", xs[rows, cols], xr[u][:], reads=[b_xr[u]], writes=[b_xs[t]])

                R = ffn_alloc(S1, N)
                ffn_block(k, R, xT, b_xT, N, W["w_ffn_gate"][l // 2], W["w_ffn_up"][l // 2], W["w_ffn_down"][l // 2],
                          act, [b_act], epi)


def stage_moe(k, C, l, W, xs, b_xs, SC, dbg=None, out=None):
    NI = 16
    with Stage(k) as S:
        w8 = S.sb("w8", [128, NI, 8], F32); b_w8 = Buf()
        dest = S.sb("dest", [128, NI, 8], F32); b_dest = Buf()
        maskb = S.sb("maskb", [128, NI, 8], BF16); b_maskb = Buf()
        maskf = S.sb("maskf", [128, NI, 8], F32); b_maskf = Buf()
        iota = S.sb("iota", [128, CAP], F32); b_iota = Buf()
        ssF = [S.sb("ssF", [128, 1], F32) for _ in range(2)]; b_ssF = bufs(2)
        k.dma("sp", iota[:], C["iota"].broadcast_to([128, CAP]), writes=[b_iota])
        with Stage(k) as S0:
            gbc = S0.sb("gbc", [128, D], F32); b_g = Buf()
            k.dma("sp", gbc[:], W["norm_ffn_g"][l:l + 1, :].broadcast_to([128, D]), writes=[b_g])
            xt = [S0.sb("xt", [128, D], F32) for _ in range(2)]; b_xt = bufs(2)
            hf = [S0.sb("hf", [128, D], F32) for _ in range(2)]; b_hf = bufs(2)
            hb = [S0.sb("hb", [128, D], BF16) for _ in range(2)]; b_hb = bufs(2)
            junk = S0.sb("junk", [128, D], BF16); b_junk = Buf()
            ss = [S0.sb("ss", [128, 1], F32) for _ in range(2)]; b_ss = bufs(2)
            wr = S0.sb("wr", [128, 16, 8], F32); b_wr = Buf()
            br = S0.sb("br", [128, 8], F32); b_br = Buf()
            k.dma("sp", wr[:], W["w_router"][0].rearrange("(kc p) e -> p kc e", p=128), writes=[b_wr])
            k.dma("sp", br[:], W["b_router"][0:1, :].broadcast_to([128, 8]), writes=[b_br])
            hfT = [S0.sb("hfT", [128, 16, 128], F32) for _ in range(2)]; b_hfT = bufs(2)
            ptf = [S0.ps("ptf", [128, 4, 128], F32) for _ in range(2)]; b_ptf = bufs(2)
            plg = [S0.ps("plg", [128, 8], F32) for _ in range(2)]; b_plg = bufs(2)
            lg = [S0.sb("lg", [128, 8], F32) for _ in range(2)]; b_lg = bufs(2)
            mx8 = [S0.sb("mx8", [128, 8], F32) for _ in range(2)]; b_mx8 = bufs(2)
            ex = [S0.sb("ex", [128, 8], F32) for _ in range(2)]; b_ex = bufs(2)
            ntop = [S0.sb("ntop", [128, 1], F32) for _ in range(2)]; b_ntop = bufs(2)
            den = [S0.sb("den", [128, 1], F32) for _ in range(2)]; b_den = bufs(2)
            b_h2b = SC["b_h2b"]
            def m0a(i):
                s = i % 2
                t = i + 1
                k.dma("sp", xt[s][:], xs[t * 128:(t + 1) * 128, :], reads=[b_xs[t]], writes=[b_xt[s]])
                k.op("act", lambda e: e.activation(out=junk[:], in_=xt[s][:], func=AF.Square, accum_out=ss[s][:]),
                     reads=[b_xt[s]], writes=[b_junk, b_ss[s]])
                k.op("act", lambda e: e.activation(out=ss[s][:], in_=ss[s][:], func=AF.Sqrt, scale=1.0 / D, bias=EPS),
                     reads=[b_ss[s]], writes=[b_ss[s]])
                k.op("dve", lambda e: e.reciprocal(out=ss[s][:], in_=ss[s][:]), reads=[b_ss[s]], writes=[b_ss[s]])
                k.op("dve", lambda e: e.scalar_tensor_tensor(out=hf[s][:], in0=xt[s][:], scalar=ss[s][:], in1=gbc[:],
                                                             op0=ALU.mult, op1=ALU.mult),
                     reads=[b_xt[s], b_ss[s], b_g], writes=[b_hf[s]])
                k.op("act", lambda e: e.activation(out=hb[s][:], in_=hf[s][:], func=AF.Copy), reads=[b_hf[s]], writes=[b_hb[s]])
                k.dma("sp", SC["h2b"][:, :, i, :].rearrange("c p f -> p c f"), hb[s][:].rearrange("p (c f) -> p c f", f=128),
                      reads=[b_hb[s]], writes=[b_h2b])

            def m0b(i):
                s = i % 2
                for c4 in range(4):
                    u = c4 % 2
                    for c in range(4):
                        cc = c4 * 4 + c
                        k.op("pe", lambda e: e.transpose(out=ptf[u][:, c, :], in_=hf[s][:, cc * 128:(cc + 1) * 128],
                                                         identity=C["identf"][:]),
                             reads=[b_hf[s], C["b"]], writes=[b_ptf[u]])
                    evac(k, "act" if u == 0 else "dve", hfT[s][:, c4 * 4:(c4 + 1) * 4, :], ptf[u][:], [b_ptf[u]], [b_hfT[s]])
                for kc in range(16):
                    k.op("pe", lambda e: e.matmul(plg[s][:], lhsT=hfT[s][:, kc, :], rhs=wr[:, kc, :], start=(kc == 0), stop=(kc == 15)),
                         reads=[b_hfT[s], b_wr], writes=[b_plg[s]])
                k.op("dve", lambda e: e.tensor_tensor(out=lg[s][:], in0=plg[s][:], in1=br[:], op=ALU.add),
                     reads=[b_plg[s], b_br], writes=[b_lg[s]])
                k.op("dve", lambda e: e.max(out=mx8[s][:], in_=lg[s][:]), reads=[b_lg[s]], writes=[b_mx8[s]])
                k.op("dve", lambda e: e.tensor_scalar(out=maskf[:, i, :], in0=lg[s][:], scalar1=mx8[s][:, 1:2], scalar2=None,
                                                      op0=ALU.is_ge), reads=[b_lg[s], b_mx8[s]], writes=[b_maskf])
                k.op("dve", lambda e: e.tensor_scalar(out=ntop[s][:], in0=mx8[s][:, 0:1], scalar1=-1.0, scalar2=None, op0=ALU.mult),
                     reads=[b_mx8[s]], writes=[b_ntop[s]])
                k.op("act", lambda e: e.activation(out=ex[s][:], in_=lg[s][:], func=AF.Exp, bias=ntop[s][:]),
                     reads=[b_lg[s], b_ntop[s]], writes=[b_ex[s]])
                k.op("dve", lambda e: e.tensor_tensor(out=ex[s][:], in0=ex[s][:], in1=maskf[:, i, :], op=ALU.mult),
                     reads=[b_ex[s], b_maskf], writes=[b_ex[s]])
                k.op("dve", lambda e: e.reduce_sum(out=den[s][:], in_=ex[s][:], axis=AX.X), reads=[b_ex[s]], writes=[b_den[s]])
                k.op("dve", lambda e: e.reciprocal(out=den[s][:], in_=den[s][:]), reads=[b_den[s]], writes=[b_den[s]])
                k.op("dve", lambda e: e.tensor_scalar(out=w8[:, i, :], in0=ex[s][:], scalar1=den[s][:], scalar2=None, op0=ALU.mult),
                     reads=[b_ex[s], b_den[s]], writes=[b_w8])
                k.op("dve", lambda e: e.tensor_copy(out=maskb[:, i, :], in_=maskf[:, i, :]), reads=[b_maskf], writes=[b_maskb])

            for i in range(NI):
                m0a(i)
                if i >= 1:
                    m0b(i - 1)
            m0b(NI - 1)
        if dbg is not None:
            k.dma("sp", dbg, maskf[:], reads=[b_maskf], writes=[Buf()])
        with Stage(k) as S1:
            onesb = S1.sb("onesb", [128, 128], BF16); b_ones = Buf()
            trib = S1.sb("trib", [128, 128], BF16); b_trib = Buf()
            k.op("dve", lambda e: e.memset(onesb[:], 1.0), writes=[b_ones])
            k.dma("sp", trib[:], C["trib"], writes=[b_trib])
            pcs = [S1.ps("pcs", [128, 8], F32) for _ in range(2)]; b_pcs = bufs(2)
            for i in range(NI):
                u = i % 2
                for i2 in range(i):
                    k.op("pe", lambda e: e.matmul(pcs[u][:], lhsT=onesb[:], rhs=maskb[:, i2, :], start=(i2 == 0), stop=False),
                         reads=[b_ones, b_maskb], writes=[b_pcs[u]])
                k.op("pe", lambda e: e.matmul(pcs[u][:], lhsT=trib[:], rhs=maskb[:, i, :], start=(i == 0), stop=True),
                     reads=[b_trib, b_maskb], writes=[b_pcs[u]])
                k.op("dve", lambda e: e.tensor_tensor(out=dest[:, i, :], in0=pcs[u][:], in1=maskf[:, i, :], op=ALU.mult),
                     reads=[b_pcs[u], b_maskf], writes=[b_dest])
            k.op("dve", lambda e: e.tensor_scalar(out=dest[:], in0=dest[:], scalar1=-1.0, scalar2=None, op0=ALU.add),
                 reads=[b_dest], writes=[b_dest])
        NS = len(STILES)
        SelT = S.sb("SelT", [128, NS, NI * 128], BF16); b_SelT = Buf()
        xy = S.sb("mxy", [128, max(16 * CAP, NS * D)], BF16); b_xT = Buf()
        xT = xy[:, 0:16 * CAP].rearrange("p (c n) -> p c n", n=CAP)
        ye = xy[:, 0:NS * D].rearrange("p (s d) -> p s d", d=D)
        b_ye = b_xT
        araw = S.sb("mact", [128, NFF * CAP], BF16)
        act = araw[:].rearrange("p (f n) -> p f n", n=CAP)
        o1 = NI * CAP
        Sel = araw[:, 0:o1].rearrange("p (i n) -> p i n", n=CAP)
        hfc = [araw[:, o1 + q * 2048:o1 + (q + 1) * 2048].rearrange("p (i f) -> p i f", f=128) for q in range(2)]
        o2 = o1 + 2 * 2048
        NXR = 3
        assert o2 + NXR * 2 * D <= NFF * CAP
        xr = [araw[:, o2 + q * 2 * D:o2 + (q + 1) * 2 * D].bitcast(F32) for q in range(NXR)]
        b_R1, b_R3 = Buf(), Buf()
        b_hfc = bufs(2); b_xr = bufs(NXR)
        b_act = [b_R1, b_R3] + b_hfc + b_xr
        R = ffn_alloc(S, CAP)
        ptr = [S.ps("ptrs", [128, 8, 128], BF16) for _ in range(2)]; b_ptr = bufs(2)
        psc = [R["psG"][0], R["psG"][1], R["psU"][0], R["psU"][1]]
        b_psc = [R["b_psG"][0], R["b_psG"][1], R["b_psU"][0], R["b_psU"][1]]
        jstep = -(-CAP // (-(-CAP // 512)))
        jsub = [(i, min(jstep, CAP - i)) for i in range(0, CAP, jstep)]
        itc = [0, 0, 0]
        for ex_i in range(NE):
            for i in range(NI):
                k.op("dve", lambda e: e.tensor_scalar(out=Sel[:, i, :], in0=iota[:], scalar1=dest[:, i, ex_i:ex_i + 1],
                                                      scalar2=None, op0=ALU.is_equal),
                     reads=[b_iota, b_dest], writes=[b_R1])
            for fc in range(16):
                s = fc % 2
                k.dma("sp", hfc[s], SC["h2b"][fc], reads=[SC["b_h2b"]], writes=[b_hfc[s]])
                for (j0, jn) in jsub:
                    u = itc[0] % 2; itc[0] += 1
                    pg, b_pg = R["psG"][u], R["b_psG"][u]
                    for i in range(NI):
                        k.op("pe", lambda e: e.matmul(pg[:, :jn], lhsT=hfc[s][:, i, :], rhs=Sel[:, i, j0:j0 + jn],
                                                      start=(i == 0), stop=(i == NI - 1)),
                             reads=[b_hfc[s], b_R1], writes=[b_pg])
                    evac(k, "act" if u == 0 else "dve", xT[:, fc, j0:j0 + jn], pg[:, :jn], [b_pg], [b_xT])
            for si, (s0, sn) in enumerate(STILES):
                for half in range(2):
                    u = itc[1] % 2; itc[1] += 1
                    for i8 in range(8):
                        i = half * 8 + i8
                        k.op("pe", lambda e: e.transpose(out=ptr[u][:sn, i8, :], in_=Sel[:, i, s0:s0 + sn], identity=C["identb"][:]),
                             reads=[b_R1, C["b"]], writes=[b_ptr[u]])
                    evac(k, "act" if u == 0 else "dve", SelT[:sn, si, half * 1024:(half + 1) * 1024],
                         ptr[u][:sn].rearrange("p a b -> p (a b)"), [b_ptr[u]], [b_SelT])

            def epi(ti, cb, ps, b_ps):
                sn = STILES[ti][1]
                evac(k, "act" if (ti + cb) % 2 == 0 else "dve", ye[:sn, ti, cb * 256:(cb + 1) * 256], ps, [b_ps], [b_ye])

            ffn_block(k, R, xT, b_xT, CAP, W["w_moe_gate"][0, ex_i], W["w_moe_up"][0, ex_i], W["w_moe_down"][0, ex_i],
                      act, b_act, epi)
            fuse_final = (out is not None and ex_i == NE - 1)
            if fuse_final:
                gF = R["wd"][1][:].rearrange("p a b -> p (a b)").bitcast(F32)[:, 0:D]
                jF = R["wd"][0][:].rearrange("p a b -> p (a b)")[:, 0:D]
                b_gF, b_jF = R["b_wd"][1], R["b_wd"][0]
                k.dma("sp", gF, W["final_norm_g"][0:1, :].broadcast_to([128, D]), writes=[b_gF])
                b_outF = Buf()
            for i in range(NI):
                t = i + 1
                u = itc[2] % NXR; itc[2] += 1
                rows = slice(t * 128, (t + 1) * 128)
                k.dma("sp", xr[u], xs[rows, :], reads=[b_xs[t]], writes=[b_xr[u]])
                for cb in range(4):
                    cols = slice(cb * 512, (cb + 1) * 512)
                    for si, (s0, sn) in enumerate(STILES):
                        k.op("pe", lambda e: e.matmul(psc[cb][:], lhsT=SelT[:sn, si, i * 128:(i + 1) * 128], rhs=ye[:sn, si, cols],
                                                      start=(si == 0), stop=(si == NS - 1)),
                             reads=[b_SelT, b_ye], writes=[b_psc[cb]])
                    k.op("dve", lambda e: e.scalar_tensor_tensor(out=xr[u][:, cols], in0=psc[cb][:], scalar=w8[:, i, ex_i:ex_i + 1],
                                                                 in1=xr[u][:, cols], op0=ALU.mult, op1=ALU.add),
                         reads=[b_psc[cb], b_w8, b_xr[u]], writes=[b_xr[u]])
                if not fuse_final:
                    k.dma("act", xs[rows, :], xr[u], reads=[b_xr[u]], writes=[b_xs[t]])
                else:
                    sF = ssF[i % 2]; b_sF = b_ssF[i % 2]
                    k.op("act", lambda e: e.activation(out=jF, in_=xr[u], func=AF.Square, accum_out=sF[:]),
                         reads=[b_xr[u]], writes=[b_jF, b_sF])
                    k.op("act", lambda e: e.activation(out=sF[:], in_=sF[:], func=AF.Sqrt, scale=1.0 / D, bias=EPS),
                         reads=[b_sF], writes=[b_sF])
                    k.op("dve", lambda e: e.reciprocal(out=sF[:], in_=sF[:]), reads=[b_sF], writes=[b_sF])
                    k.op("dve", lambda e: e.scalar_tensor_tensor(out=xr[u], in0=xr[u], scalar=sF[:], in1=gF, op0=ALU.mult, op1=ALU.mult),
                         reads=[b_xr[u], b_sF, b_gF], writes=[b_xr[u]])
                    k.dma("act", out[i * 128:(i + 1) * 128, :], xr[u], reads=[b_xr[u]], writes=[b_outF])


def stage_final(k, C, W, xs, b_xs, out):
    with Stage(k) as S:
        gbc = S.sb("gbc", [128, D], F32); b_g = Buf()
        k.dma("sp", gbc[:], W["final_norm_g"][0:1, :].broadcast_to([128, D]), writes=[b_g])
        xt = [S.sb("xt", [128, D], F32) for _ in range(2)]; b_xt = bufs(2)
        ot = [S.sb("ot", [128, D], F32) for _ in range(2)]; b_ot = bufs(2)
        junk = S.sb("junk", [128, D], BF16); b_junk = Buf()
        ss = [S.sb("ss", [128, 1], F32) for _ in range(2)]; b_ss = bufs(2)
        b_out = Buf()
        for i in range(16):
            s = i % 2
            t = i + 1
            k.dma("sp", xt[s][:], xs[t * 128:(t + 1) * 128, :], reads=[b_xs[t]], writes=[b_xt[s]])
            k.op("act", lambda e: e.activation(out=junk[:], in_=xt[s][:], func=AF.Square, accum_out=ss[s][:]),
                 reads=[b_xt[s]], writes=[b_junk, b_ss[s]])
            k.op("act", lambda e: e.activation(out=ss[s][:], in_=ss[s][:], func=AF.Sqrt, scale=1.0 / D, bias=EPS),
                 reads=[b_ss[s]], writes=[b_ss[s]])
            k.op("dve", lambda e: e.reciprocal(out=ss[s][:], in_=ss[s][:]), reads=[b_ss[s]], writes=[b_ss[s]])
            k.op("dve", lambda e: e.scalar_tensor_tensor(out=ot[s][:], in0=xt[s][:], scalar=ss[s][:], in1=gbc[:],
                                                         op0=ALU.mult, op1=ALU.mult),
                 reads=[b_xt[s], b_ss[s], b_g], writes=[b_ot[s]])
            k.dma("sp", out[i * 128:(i + 1) * 128, :], ot[s][:], reads=[b_ot[s]], writes=[b_out])


ALL_STAGES = ["init", "A0", "attn0", "ml0", "C0", "ffn0", "A1", "attn1", "ml1", "C1", "moe"]
_NC_CACHE = {}


def kernel(**inputs):
    f32 = lambda a: np.ascontiguousarray(np.asarray(a, dtype=np.float32))
    x = f32(inputs["x"])
    B = x.shape[0]
    shared = {}
    for name in W_SHAPES:
        if name == "conv_pk":
            shared[name] = pack_conv(f32(inputs["conv_w"]), f32(inputs["conv_b"]))
        elif name == "final_norm_g":
            shared[name] = f32(inputs["final_norm_g"]).reshape(1, D)
        else:
            shared[name] = f32(inputs[name])
    shared["meta"] = f32(inputs["meta_tokens"])
    c = make_consts()
    gp, gc, gm = make_gathers(f32(inputs["rel_bias_table"]))
    c["g_prev"], c["g_cur"], c["g_meta"] = gp, gc, gm
    for n, v in c.items():
        shared["c_" + n] = v
    if "nc" not in _NC_CACHE:
        _NC_CACHE["nc"] = build(ALL_STAGES)
    nc = _NC_CACHE["nc"]
    in_maps = []
    for b in range(B):
        m = dict(shared)
        m["x"] = x[b]
        in_maps.append(m)
    res = run_bass_kernel_spmd(nc, in_maps, core_ids=list(range(B)))
    _NC_CACHE["counts"] = [np.asarray(r["dbg_mask"]).sum(axis=(0, 1)) for r in res.results]
    return np.stack([np.asarray(r["out"], dtype=np.float32) for r in res.results], axis=0)
```
